# Optimizing a Trainium2 kernel written in Bass

```python
import math
import jax
import jax.numpy as jnp
from jax import lax
import numpy as np

D_MODEL = 2048
BATCH = 2
SEQ = 8192
DEPTH = 4

GRID_W = 64
CTX_LEN = 256
N_MIXERS = 3
N_A = (DEPTH + 2) // 3
N_B = (DEPTH + 1) // 3
N_C = DEPTH // 3
DEEPNORM_ALPHA = (2 * DEPTH) ** 0.25
DEEPNORM_BETA = (8 * DEPTH) ** -0.25
LN_EPS = 1e-5

RWKV_HEAD = 64
RWKV_HEADS = D_MODEL // RWKV_HEAD
R_DECAY = max(32, int(round(1.8 * D_MODEL ** 0.5 / 32)) * 32)
R_ICLR = max(32, int(round(1.8 * D_MODEL ** 0.5 / 32)) * 32)
R_VRES = max(32, int(round(1.3 * D_MODEL ** 0.5 / 32)) * 32)
R_GATE = max(32, int(round(0.6 * D_MODEL ** 0.8 / 32)) * 32)
GN_EPS = 64e-5

D_RNN = D_MODEL
LRU_BLOCKS = 8
LRU_BW = D_RNN // LRU_BLOCKS
CONV_W = 4
LRU_C = 8.0

HEAD_DIM = 64
Q_HEADS = D_MODEL // HEAD_DIM
KV_HEADS = Q_HEADS // 8
GROUP = Q_HEADS // KV_HEADS
Q_DIM = Q_HEADS * HEAD_DIM
KV_DIM = KV_HEADS * HEAD_DIM
WINDOW = 128
ATT_BLOCK = WINDOW
ROPE_THETA = 10000.0
NEG_INF = -1e30

N_EXPERTS = 32
TOP_K = 4
D_EXPERT = 3 * D_MODEL // 8
SWIGLU_LIMIT = 7.0
SWIGLU_ALPHA = 1.702
MOE_BLOCK = 256

kernel_name = 'hybrid_rwkv7_rglru_swa_moe_diffusion_trunk'


def _layer_norm(x, g, b):
    xf = x.astype(jnp.float32)
    mu = jnp.mean(xf, axis=-1, keepdims=True)
    var = jnp.mean(jnp.square(xf - mu), axis=-1, keepdims=True)
    return ((xf - mu) * lax.rsqrt(var + LN_EPS) * g.astype(jnp.float32) + b.astype(jnp.float32)).astype(x.dtype)


def _modulate(x, shift, scale):
    return x * (1.0 + scale) + shift


def _axial_rope(rows):
    half = HEAD_DIM // 2
    inv = ROPE_THETA ** (-jnp.arange(0, half, 2, dtype=jnp.float32) / half)
    row = jnp.repeat(jnp.arange(rows, dtype=jnp.float32), GRID_W)
    col = jnp.tile(jnp.arange(GRID_W, dtype=jnp.float32), rows)
    ang_r = row[:, None] * inv[None, :]
    ang_c = col[:, None] * inv[None, :]
    ang = jnp.concatenate([ang_r, ang_r, ang_c, ang_c], axis=-1)
    return jnp.cos(ang), jnp.sin(ang)


def _rotate_axial(x):
    def rot_half(p):
        p1, p2 = jnp.split(p, 2, axis=-1)
        return jnp.concatenate([-p2, p1], axis=-1)
    xr, xc = jnp.split(x, 2, axis=-1)
    return jnp.concatenate([rot_half(xr), rot_half(xc)], axis=-1)


def _apply_rope(x, cos, sin):
    shape = (1, x.shape[1]) + (1,) * (x.ndim - 3) + (HEAD_DIM,)
    return x * cos.reshape(shape).astype(x.dtype) + _rotate_axial(x) * sin.reshape(shape).astype(x.dtype)


def _token_shift(h):
    hp = jnp.pad(h, ((0, 0), (1, 1), (0, 0)))
    return 0.5 * (hp[:, :-2] + hp[:, 2:]) - h


def _dir_time_major(t):
    t = t.astype(jnp.float32)
    fwd, bwd = (t, t) if t.ndim == 3 else (t[0], t[1])
    s = jnp.stack([fwd, jnp.flip(bwd, axis=1)], axis=0)
    _, b, n, _ = s.shape
    return jnp.transpose(s.reshape(2, b, n, RWKV_HEADS, RWKV_HEAD), (2, 0, 1, 3, 4))


def _wkv7_scan(inputs, s0):
    def step(s, inp):
        r, w, k, v, za, zb = inp
        sa = jnp.einsum('zbhij,zbhj->zbhi', s, za)
        s = s * w[..., None, :] + sa[..., :, None] * zb[..., None, :] + v[..., :, None] * k[..., None, :]
        return s, jnp.einsum('zbhij,zbhj->zbhi', s, r)
    s_fin, out = lax.scan(step, s0, inputs)
    o = out[:, 0] + jnp.flip(out[:, 1], axis=0)
    return jnp.transpose(o, (1, 0, 2, 3)), s_fin


def _rwkv7_side(h, v_first, vres, mix, w_rkv, w0, w1, w2, a0, a1, a2, g1, g2, k_k, k_a):
    b, n, d = h.shape
    xx = _token_shift(h)
    xr, xw, xk, xv, xa, xg = [h + xx * mix[m] for m in range(6)]
    r = xr @ w_rkv[0]
    k = xk @ w_rkv[1]
    v = xv @ w_rkv[2]
    if vres is None:
        v_first = v
    else:
        v0, v1, v2 = vres
        v = v + (v_first - v) * jax.nn.sigmoid(v0 + (xv @ v1) @ v2)
    w_log = w0[:, None, None, :] + jnp.einsum('zbtr,zrd->zbtd', jnp.tanh(jnp.einsum('btd,zdr->zbtr', xw, w1)), w2)
    decay = jnp.exp(-jnp.exp(-jax.nn.softplus(-w_log.astype(jnp.float32)) - 0.5))
    iclr = jax.nn.sigmoid((a0[:, None, None, :] + jnp.einsum('zbtr,zrd->zbtd', jnp.einsum('btd,zdr->zbtr', xa, a1), a2)).astype(jnp.float32))
    g = jax.nn.sigmoid(xg @ g1) @ g2
    kk = (k * k_k).astype(jnp.float32).reshape(b, n, RWKV_HEADS, RWKV_HEAD)
    kk = (kk / jnp.maximum(jnp.linalg.norm(kk, axis=-1, keepdims=True), 1e-12)).reshape(b, n, d)
    k_dir = k.astype(jnp.float32)[None] * (1.0 + (iclr - 1.0) * k_a.astype(jnp.float32))
    scan_in = tuple(_dir_time_major(t) for t in (r, decay, k_dir, v, -kk, kk[None] * iclr))
    return scan_in, (r, jnp.mean(k_dir, axis=0), v, g), v_first


def _head_norm(o, g, b):
    mu = jnp.mean(o, axis=-1, keepdims=True)
    var = jnp.mean(jnp.square(o - mu), axis=-1, keepdims=True)
    y = ((o - mu) * lax.rsqrt(var + GN_EPS)).reshape(o.shape[0], o.shape[1], -1)
    return y * g.astype(jnp.float32) + b.astype(jnp.float32)


def _rwkv7_out(o, r, k_bonus, v, g, r_k, lnx_g, lnx_b, w_o):
    b, n, _ = r.shape
    hs = (b, n, RWKV_HEADS, RWKV_HEAD)
    bonus = jnp.sum(r.reshape(hs).astype(jnp.float32) * k_bonus.reshape(hs) * r_k.astype(jnp.float32), axis=-1, keepdims=True) * v.reshape(hs).astype(jnp.float32)
    y = _head_norm(o, lnx_g, lnx_b) + bonus.reshape(b, n, -1)
    return (y.astype(g.dtype) * g) @ w_o


def _rwkv7_mixer(h_lat, h_ctx, vf_lat, vf_ctx, vres, mix, w_rkv, w0, w1, w2, a0, a1, a2, g1, g2, k_k, k_a, r_k, lnx_g, lnx_b, w_o, ctx_out):
    common = (mix, w_rkv, w0, w1, w2, a0, a1, a2, g1, g2, k_k, k_a)
    scan_c, aux_c, vf_ctx = _rwkv7_side(h_ctx, vf_ctx, vres, *common)
    scan_l, aux_l, vf_lat = _rwkv7_side(h_lat, vf_lat, vres, *common)
    s0 = jnp.zeros((2, h_lat.shape[0], RWKV_HEADS, RWKV_HEAD, RWKV_HEAD), jnp.float32)
    o_c, s_ctx = _wkv7_scan(scan_c, s0)
    o_l, _ = _wkv7_scan(scan_l, s_ctx)
    tail = (r_k, lnx_g, lnx_b, w_o)
    y_lat = _rwkv7_out(o_l, *aux_l, *tail)
    y_ctx = _rwkv7_out(o_c, *aux_c, *tail) if ctx_out else None
    return y_lat, y_ctx, vf_lat, vf_ctx


def _dwconv(x, w, b):
    y = lax.conv_general_dilated(x, w[:, None, :], window_strides=(1,), padding=[(CONV_W // 2, CONV_W - 1 - CONV_W // 2)], dimension_numbers=('NWC', 'WIO', 'NWC'), feature_group_count=x.shape[-1])
    return y + b


def _rglru_coeffs(x, gate_w, gate_b, lam):
    b, n, _ = x.shape
    xb = x.reshape(b, n, LRU_BLOCKS, LRU_BW)
    pre = jnp.einsum('btnc,zgncd->zgbtnd', xb, gate_w).reshape(2, 2, b, n, D_RNN) + gate_b[:, :, None, None, :]
    gates = jax.nn.sigmoid(pre.astype(jnp.float32))
    log_a = LRU_C * gates[:, 0] * jax.nn.log_sigmoid(lam.astype(jnp.float32))[:, None, None, :]
    a = jnp.exp(log_a)
    u = jnp.sqrt(-jnp.expm1(2.0 * log_a)) * (gates[:, 1] * x.astype(jnp.float32)[None])
    return a, u


def _linear_scan(a, u, h0, reverse):
    if h0 is not None:
        edge = -1 if reverse else 0
        u = u.at[:, edge].add(a[:, edge] * h0)
    _, h = lax.associative_scan(lambda l, r: (l[0] * r[0], r[0] * l[1] + r[1]), (a, u), axis=1, reverse=reverse)
    return h


def _rglru_mixer(h_lat, h_ctx, w_in, conv_w, conv_b, gate_w, gate_b, lam, w_out, ctx_out):
    def prep(h):
        gelu_in, rnn_in = jnp.split(h @ w_in, 2, axis=-1)
        a, u = _rglru_coeffs(_dwconv(rnn_in, conv_w, conv_b), gate_w, gate_b, lam)
        return jax.nn.gelu(gelu_in), a, u
    y_c, a_c, u_c = prep(h_ctx)
    y_l, a_l, u_l = prep(h_lat)
    hf_c = _linear_scan(a_c[0], u_c[0], None, False)
    hb_c = _linear_scan(a_c[1], u_c[1], None, True)
    hf_l = _linear_scan(a_l[0], u_l[0], hf_c[:, -1], False)
    hb_l = _linear_scan(a_l[1], u_l[1], hb_c[:, 0], True)
    out_l = ((hf_l + hb_l).astype(y_l.dtype) * y_l) @ w_out
    out_c = ((hf_c + hb_c).astype(y_c.dtype) * y_c) @ w_out if ctx_out else None
    return out_l, out_c


def _sink_softmax(s, sink_hg):
    sk = jnp.broadcast_to(sink_hg[None, :, :, None, None], s.shape[:-1] + (1,))
    p = jax.nn.softmax(jnp.concatenate([s, sk], axis=-1), axis=-1)
    return p[..., :-1]


def _swa_mixer(h_lat, h_ctx, w_qkv, b_qkv, sink, w_o, b_o, cos, sin, ctx_out):
    bsz, n_lat, _ = h_lat.shape
    scale = HEAD_DIM ** -0.5

    def proj(h):
        n = h.shape[1]
        q, k, v = jnp.split(h @ w_qkv + b_qkv, [Q_DIM, Q_DIM + KV_DIM], axis=-1)
        return (q.reshape(bsz, n, KV_HEADS, GROUP, HEAD_DIM), k.reshape(bsz, n, KV_HEADS, HEAD_DIM), v.reshape(bsz, n, KV_HEADS, HEAD_DIM))

    q_c, k_c, v_c = proj(h_ctx)
    q_l, k_l, v_l = proj(h_lat)
    q_l = _apply_rope(q_l, cos, sin)
    k_l = _apply_rope(k_l, cos, sin)
    sink_hg = sink.reshape(KV_HEADS, GROUP).astype(jnp.float32)
    pad = ((0, 0), (ATT_BLOCK, ATT_BLOCK), (0, 0), (0, 0))
    kp = jnp.pad(k_l, pad)
    vp = jnp.pad(v_l, pad)

    def block(bi):
        start = bi * ATT_BLOCK
        qb = lax.dynamic_slice_in_dim(q_l, start, ATT_BLOCK, axis=1)
        kb = lax.dynamic_slice_in_dim(kp, start, 3 * ATT_BLOCK, axis=1)
        vb = lax.dynamic_slice_in_dim(vp, start, 3 * ATT_BLOCK, axis=1)
        qpos = start + jnp.arange(ATT_BLOCK)
        kpos = start - ATT_BLOCK + jnp.arange(3 * ATT_BLOCK)
        valid = (jnp.abs(kpos[None, :] - qpos[:, None]) <= WINDOW) & (kpos >= 0)[None, :] & (kpos < n_lat)[None, :]
        s_lat = jnp.where(valid, jnp.einsum('bqhgd,bkhd->bhgqk', qb, kb).astype(jnp.float32) * scale, NEG_INF)
        s_ctx = jnp.einsum('bqhgd,bchd->bhgqc', qb, k_c).astype(jnp.float32) * scale
        p = _sink_softmax(jnp.concatenate([s_lat, s_ctx], axis=-1), sink_hg).astype(v_l.dtype)
        return (jnp.einsum('bhgqk,bkhd->bqhgd', p[..., :3 * ATT_BLOCK], vb) + jnp.einsum('bhgqc,bchd->bqhgd', p[..., 3 * ATT_BLOCK:], v_c))

    o = lax.map(block, jnp.arange(n_lat // ATT_BLOCK))
    y_lat = jnp.moveaxis(o, 0, 1).reshape(bsz, n_lat, Q_DIM) @ w_o + b_o
    y_ctx = None
    if ctx_out:
        s = jnp.einsum('bqhgd,bchd->bhgqc', q_c, k_c).astype(jnp.float32) * scale
        p = _sink_softmax(s, sink_hg).astype(v_c.dtype)
        y_ctx = jnp.einsum('bhgqc,bchd->bqhgd', p, v_c).reshape(bsz, h_ctx.shape[1], Q_DIM) @ w_o + b_o
    return y_lat, y_ctx


def _clamped_swiglu(hid):
    g, u = jnp.split(hid, 2, axis=-1)
    g = jnp.minimum(g, SWIGLU_LIMIT)
    u = jnp.clip(u, -SWIGLU_LIMIT, SWIGLU_LIMIT)
    return g * jax.nn.sigmoid(SWIGLU_ALPHA * g) * (u + 1.0)


def _moe(h, router_w, router_b, w_in, b_in, w_out, b_out):
    n, d = h.shape
    logits = (h @ router_w + router_b).astype(jnp.float32)
    top_val, top_idx = lax.top_k(logits, TOP_K)
    gates = jax.nn.softmax(top_val, axis=-1)
    nk = n * TOP_K
    e_flat = top_idx.reshape(nk)
    order = jnp.argsort(e_flat)
    e_sorted = e_flat[order]
    counts = jnp.bincount(e_flat, length=N_EXPERTS)
    padded = (counts + MOE_BLOCK - 1) // MOE_BLOCK * MOE_BLOCK
    pad_end = jnp.cumsum(padded)
    pad_start = pad_end - padded
    grp_start = jnp.cumsum(counts) - counts
    dest = pad_start[e_sorted] + jnp.arange(nk) - grp_start[e_sorted]
    n_blocks = -(-(nk + N_EXPERTS * (MOE_BLOCK - 1)) // MOE_BLOCK)
    cap = n_blocks * MOE_BLOCK
    slot_tok = jnp.zeros((cap,), jnp.int32).at[dest].set((order // TOP_K).astype(jnp.int32))
    slot_gate = jnp.zeros((cap,), jnp.float32).at[dest].set(gates.reshape(nk)[order])
    block_expert = jnp.minimum(jnp.searchsorted(pad_end, jnp.arange(n_blocks) * MOE_BLOCK, side='right'), N_EXPERTS - 1)
    xs = h[slot_tok].reshape(n_blocks, MOE_BLOCK, d)

    def expert_block(args):
        xb, e = args
        return _clamped_swiglu(xb @ w_in[e] + b_in[e]) @ w_out[e] + b_out[e]

    ys = lax.map(expert_block, (xs, block_expert)).reshape(cap, d)
    return jnp.zeros_like(h).at[slot_tok].add(ys * slot_gate[:, None].astype(ys.dtype))


def setup_inputs(seed: int = 0) -> dict:
    key = jax.random.key(seed)
    ks = iter(jax.random.split(key, 64))

    def nrm(shape, scale):
        return scale * jax.random.normal(next(ks), shape, jnp.float32)

    D = D_MODEL
    inp = {}
    inp['x'] = nrm((BATCH, SEQ, D), 1.0)
    inp['c'] = nrm((BATCH, D), 1.0)
    inp['ctx'] = nrm((BATCH, CTX_LEN, D), 1.0)
    inp['c_ctx'] = nrm((D,), 1.0)
    inp['ada_w'] = nrm((DEPTH, D, 6 * D), 0.5 * D ** -0.5)
    inp['ada_b'] = nrm((DEPTH, 6 * D), 0.01)
    inp['ln_g'] = 1.0 + nrm((DEPTH, 2, D), 0.02)
    inp['ln_b'] = nrm((DEPTH, 2, D), 0.02)
    inp['rwkv_mix'] = jax.random.uniform(next(ks), (N_A, 6, D), jnp.float32)
    inp['rwkv_w_rkv'] = nrm((N_A, 3, D, D), D ** -0.5)
    layer_ratio = (N_MIXERS * jnp.arange(N_A, dtype=jnp.float32) / max(DEPTH - 1, 1))[:, None, None]
    chan = (jnp.arange(D, dtype=jnp.float32) / (D - 1))[None, None, :]
    decay_speed = -7.0 + 5.0 * chan ** (0.85 + jnp.sqrt(layer_ratio))
    inp['rwkv_w0'] = decay_speed + 0.5 + nrm((N_A, 2, D), 0.1)
    inp['rwkv_w1'] = nrm((N_A, 2, D, R_DECAY), D ** -0.5)
    inp['rwkv_w2'] = nrm((N_A, 2, R_DECAY, D), 0.1 * R_DECAY ** -0.5)
    inp['rwkv_a0'] = nrm((N_A, 2, D), 0.1)
    inp['rwkv_a1'] = nrm((N_A, 2, D, R_ICLR), D ** -0.5)
    inp['rwkv_a2'] = nrm((N_A, 2, R_ICLR, D), 0.3 * R_ICLR ** -0.5)
    inp['rwkv_v0'] = 1.0 + nrm((N_A - 1, D), 0.1)
    inp['rwkv_v1'] = nrm((N_A - 1, D, R_VRES), D ** -0.5)
    inp['rwkv_v2'] = nrm((N_A - 1, R_VRES, D), 0.3 * R_VRES ** -0.5)
    inp['rwkv_g1'] = nrm((N_A, D, R_GATE), D ** -0.5)
    inp['rwkv_g2'] = nrm((N_A, R_GATE, D), R_GATE ** -0.5)
    inp['rwkv_k_k'] = 0.85 + nrm((N_A, D), 0.02)
    inp['rwkv_k_a'] = 1.0 + nrm((N_A, D), 0.02)
    inp['rwkv_r_k'] = nrm((N_A, RWKV_HEADS, RWKV_HEAD), 0.1)
    inp['rwkv_lnx_g'] = 1.0 + nrm((N_A, D), 0.02)
    inp['rwkv_lnx_b'] = nrm((N_A, D), 0.02)
    inp['rwkv_w_o'] = nrm((N_A, D, D), DEEPNORM_BETA * D ** -0.5)
    inp['lru_w_in'] = nrm((N_B, D, 2 * D_RNN), D ** -0.5)
    inp['lru_conv_w'] = nrm((N_B, CONV_W, D_RNN), CONV_W ** -0.5)
    inp['lru_conv_b'] = nrm((N_B, D_RNN), 0.01)
    inp['lru_gate_w'] = nrm((N_B, 2, 2, LRU_BLOCKS, LRU_BW, LRU_BW), LRU_BW ** -0.5)
    inp['lru_gate_b'] = nrm((N_B, 2, 2, D_RNN), 0.01)
    a_pow = jax.random.uniform(next(ks), (N_B, 2, D_RNN), jnp.float32, 0.9, 0.999)
    a_base = a_pow ** (1.0 / LRU_C)
    inp['lru_lambda'] = jnp.log(a_base) - jnp.log1p(-a_base)
    inp['lru_w_out'] = nrm((N_B, D_RNN, D), DEEPNORM_BETA * D_RNN ** -0.5)
    inp['attn_w_qkv'] = nrm((N_C, D, Q_DIM + 2 * KV_DIM), D ** -0.5)
    inp['attn_b_qkv'] = nrm((N_C, Q_DIM + 2 * KV_DIM), 0.01)
    inp['attn_sink'] = nrm((N_C, Q_HEADS), 1.0)
    inp['attn_w_o'] = nrm((N_C, Q_DIM, D), DEEPNORM_BETA * Q_DIM ** -0.5)
    inp['attn_b_o'] = nrm((N_C, D), 0.01)
    inp['router_w'] = nrm((DEPTH, D, N_EXPERTS), D ** -0.5)
    inp['router_b'] = nrm((DEPTH, N_EXPERTS), 0.01)
    inp['moe_w_in'] = nrm((DEPTH, N_EXPERTS, D, 2 * D_EXPERT), D ** -0.5)
    inp['moe_b_in'] = nrm((DEPTH, N_EXPERTS, 2 * D_EXPERT), 0.01)
    inp['moe_w_out'] = nrm((DEPTH, N_EXPERTS, D_EXPERT, D), DEEPNORM_BETA * D_EXPERT ** -0.5)
    inp['moe_b_out'] = nrm((DEPTH, N_EXPERTS, D), 0.01)
    return inp


def reference(x, c, ctx, c_ctx, ada_w, ada_b, ln_g, ln_b,
              rwkv_mix, rwkv_w_rkv, rwkv_w0, rwkv_w1, rwkv_w2, rwkv_a0, rwkv_a1, rwkv_a2,
              rwkv_v0, rwkv_v1, rwkv_v2, rwkv_g1, rwkv_g2, rwkv_k_k, rwkv_k_a, rwkv_r_k,
              rwkv_lnx_g, rwkv_lnx_b, rwkv_w_o,
              lru_w_in, lru_conv_w, lru_conv_b, lru_gate_w, lru_gate_b, lru_lambda, lru_w_out,
              attn_w_qkv, attn_b_qkv, attn_sink, attn_w_o, attn_b_o,
              router_w, router_b, moe_w_in, moe_b_in, moe_w_out, moe_b_out):
    bsz, n_lat, d = x.shape
    rows = n_lat // GRID_W
    cos, sin = _axial_rope(rows)
    cond_lat = jax.nn.silu(c)
    cond_ctx = jax.nn.silu(c_ctx)
    x_lat, x_ctx = x, ctx
    vf_lat = None
    vf_ctx = None
    for i in range(DEPTH):
        last = i == DEPTH - 1
        kind, j = i % N_MIXERS, i // N_MIXERS
        mod_l = [m[:, None, :] for m in jnp.split(cond_lat @ ada_w[i] + ada_b[i], 6, axis=-1)]
        mod_c = jnp.split(cond_ctx @ ada_w[i] + ada_b[i], 6, axis=-1)
        h_lat = _modulate(x_lat, mod_l[0], mod_l[1])
        h_ctx = _modulate(x_ctx, mod_c[0], mod_c[1])
        if kind == 0:
            vres = None if j == 0 else (rwkv_v0[j - 1], rwkv_v1[j - 1], rwkv_v2[j - 1])
            y_lat, y_ctx, vf_lat, vf_ctx = _rwkv7_mixer(
                h_lat, h_ctx, vf_lat, vf_ctx, vres, rwkv_mix[j], rwkv_w_rkv[j], rwkv_w0[j], rwkv_w1[j], rwkv_w2[j],
                rwkv_a0[j], rwkv_a1[j], rwkv_a2[j], rwkv_g1[j], rwkv_g2[j], rwkv_k_k[j], rwkv_k_a[j], rwkv_r_k[j],
                rwkv_lnx_g[j], rwkv_lnx_b[j], rwkv_w_o[j], not last)
        elif kind == 1:
            y_lat, y_ctx = _rglru_mixer(h_lat, h_ctx, lru_w_in[j], lru_conv_w[j], lru_conv_b[j], lru_gate_w[j],
                                        lru_gate_b[j], lru_lambda[j], lru_w_out[j], not last)
        else:
            y_lat, y_ctx = _swa_mixer(h_lat, h_ctx, attn_w_qkv[j], attn_b_qkv[j], attn_sink[j], attn_w_o[j],
                                      attn_b_o[j], cos, sin, not last)
        x_lat = _layer_norm(DEEPNORM_ALPHA * x_lat + mod_l[2] * y_lat, ln_g[i, 0], ln_b[i, 0])
        if not last:
            x_ctx = _layer_norm(DEEPNORM_ALPHA * x_ctx + mod_c[2] * y_ctx, ln_g[i, 0], ln_b[i, 0])
        moe_p = (router_w[i], router_b[i], moe_w_in[i], moe_b_in[i], moe_w_out[i], moe_b_out[i])
        h_lat = _modulate(x_lat, mod_l[3], mod_l[4]).reshape(-1, d)
        if last:
            f_lat = _moe(h_lat, *moe_p)
        else:
            h_ctx = _modulate(x_ctx, mod_c[3], mod_c[4]).reshape(-1, d)
            f = _moe(jnp.concatenate([h_lat, h_ctx], axis=0), *moe_p)
            f_lat = f[:h_lat.shape[0]]
            x_ctx = _layer_norm(DEEPNORM_ALPHA * x_ctx + mod_c[5] * f[h_lat.shape[0]:].reshape(x_ctx.shape), ln_g[i, 1], ln_b[i, 1])
        x_lat = _layer_norm(DEEPNORM_ALPHA * x_lat + mod_l[5] * f_lat.reshape(x_lat.shape), ln_g[i, 1], ln_b[i, 1])
    return x_lat
```

```python
import numpy as np
import concourse.bass as bass
import concourse.mybir as mybir
from concourse.bass_utils import run_bass_kernel_spmd

F32 = mybir.dt.float32
BF16 = mybir.dt.bfloat16
ALU = mybir.AluOpType
AF = mybir.ActivationFunctionType
AX = mybir.AxisListType

SEM_LIMIT = 20000
N_DMA_SEMS = 12


class Trk:
    __slots__ = ("w", "r")

    def __init__(self):
        self.w = None
        self.r = {}

    def clone(self):
        t = Trk()
        t.w = self.w
        t.r = dict(self.r)
        return t


class View:
    __slots__ = ("ap", "trks")

    def __init__(self, ap, trks):
        self.ap = ap
        self.trks = trks

    def __getitem__(self, idx):
        return View(self.ap[idx], self.trks)

    def re(self, s, **kw):
        return View(self.ap.rearrange(s, **kw), self.trks)

    def bc(self, shape):
        return View(self.ap.to_broadcast(shape), self.trks)

    def with_ap(self, ap):
        return View(ap, self.trks)


class Tile:
    def __init__(self, handle):
        self.t = handle
        self.base = Trk()
        self.keys = {}

    def _all(self):
        return [self.base] + list(self.keys.values())

    def __getitem__(self, idx):
        return View(self.t[idx], self._all())

    def ap(self):
        return self.t.ap() if hasattr(self.t, "ap") and callable(self.t.ap) else self.t[:]

    def k(self, key):
        if key not in self.keys:
            self.keys[key] = self.base.clone()
        return _Keyed(self, key)

    def raw(self, ap, key=None):
        if key is None:
            return View(ap, self._all())
        self.k(key)
        return View(ap, [self.keys[key]])


class _Keyed:
    def __init__(self, tile, key):
        self.tile = tile
        self.key = key

    def __getitem__(self, idx):
        return View(self.tile.t[idx], [self.tile.keys[self.key]])


class Prog:
    def __init__(self, nc):
        self.nc = nc
        self.eng = dict(pe=nc.tensor, dve=nc.vector, act=nc.scalar, pool=nc.gpsimd, sp=nc.sync)
        self.sem = {}
        self.cnt = {}
        self.seen = {e: {} for e in self.eng}
        self.pend = {e: ([], []) for e in self.eng}
        self.nsem = 0
        self.dsems = {}
        self.dptr = {}
        self.ninst = {e: 0 for e in self.eng}
        self._uid = 0

    def new_sem(self, name):
        self.nsem += 1
        return self.nc.semaphore(f"{name}_{self.nsem}").__enter__()

    def sb(self, shape, dtype=F32, name=None):
        self._uid += 1
        return Tile(self.nc.alloc_sbuf_tensor(f"{name or 't'}_{self._uid}", list(shape), dtype))

    def ps(self, shape, dtype=F32, name=None):
        self._uid += 1
        return Tile(self.nc.alloc_psum_tensor(f"{name or 'p'}_{self._uid}", list(shape), dtype))

    def dram(self, name, shape, dtype=F32, kind="Internal"):
        return Tile(self.nc.dram_tensor(name, list(shape), dtype, kind=kind))

    def _deps(self, e, reads, writes):
        need = {}

        def add(tok):
            if tok is None:
                return
            sem, val, eng = tok
            if eng == e and eng == "pe":
                return
            if eng == e and e != "pool":
                if sem is not self.sem.get(e) or self.cnt[e] - val >= 3:
                    return
            k = id(sem)
            if k not in need or need[k][1] < val:
                need[k] = (sem, val)

        for v in reads:
            for t in v.trks:
                add(t.w)
        for v in writes:
            for t in v.trks:
                add(t.w)
                for tok in t.r.values():
                    add(tok)
        out = []
        for k, (sem, val) in need.items():
            if self.seen[e].get(k, 0) < val:
                self.seen[e][k] = val
                out.append((sem, val))
        return out

    def _record(self, tok, reads, writes):
        sem = tok[0]
        for v in reads:
            for t in v.trks:
                t.r[id(sem)] = tok
        for v in writes:
            for t in v.trks:
                t.w = tok
                t.r = {}

    def op(self, e, fn, reads=(), writes=(), inc=True):
        eng = self.eng[e]
        for sem, val in self._deps(e, reads, writes):
            eng.wait_ge(sem, val)
        ins = fn(eng)
        self.ninst[e] += 1
        pr, pw = self.pend[e]
        pr.extend(reads)
        pw.extend(writes)
        if not inc:
            return None
        if e not in self.sem or self.cnt[e] >= SEM_LIMIT:
            self.sem[e] = self.new_sem(f"s_{e}")
            self.cnt[e] = 0
        self.cnt[e] += 1
        ins.then_inc(self.sem[e], 1)
        tok = (self.sem[e], self.cnt[e], e)
        self._record(tok, pr, pw)
        self.pend[e] = ([], [])
        return tok

    def dma(self, q, out, in_, **kw):
        eng = self.eng[q]
        assert not self.pend[q][0] and not self.pend[q][1], "pending non-inc ops on DMA queue engine"
        if q not in self.dsems:
            self.dsems[q] = [[self.new_sem(f"d_{q}{i}"), 0] for i in range(N_DMA_SEMS)]
            self.dptr[q] = 0
        slot = self.dsems[q][self.dptr[q]]
        self.dptr[q] = (self.dptr[q] + 1) % N_DMA_SEMS
        deps = self._deps(q, [in_], [out])
        if slot[1] > 0 and self.seen[q].get(id(slot[0]), 0) < slot[1]:
            self.seen[q][id(slot[0])] = slot[1]
            deps.append((slot[0], slot[1]))
        for sem, val in deps:
            eng.wait_ge(sem, val)
        ins = eng.dma_start(out=out.ap, in_=in_.ap, **kw)
        slot[1] += 16
        ins.then_inc(slot[0], 16)
        tok = (slot[0], slot[1], "dma_" + q)
        self._record(tok, [in_], [out])
        self.ninst[q] += 1
        return tok

    def wait_all(self, e, views):
        eng = self.eng[e]
        for sem, val in self._deps(e, views, []):
            eng.wait_ge(sem, val)

    def mm(self, out, lhsT, rhs, start=True, stop=True, inc=None):
        if inc is None:
            inc = True
        return self.op("pe", lambda g: g.matmul(out.ap, lhsT.ap, rhs.ap, start=start, stop=stop),
                       [lhsT, rhs], [out], inc=inc)

    def mmg(self, out, lhsT, rhs, start=True, stop=True):
        return self.mm(out, lhsT, rhs, start=start, stop=stop, inc=stop)

    def tr(self, out, in_, ident):
        return self.op("pe", lambda g: g.transpose(out.ap, in_.ap, ident.ap), [in_, ident], [out])

    def act(self, out, in_, func, bias=None, scale=1.0, e="act"):
        rd = [in_]
        kw = {}
        if isinstance(bias, View):
            rd.append(bias)
            kw["bias"] = bias.ap
        elif bias is not None:
            kw["bias"] = bias
        if isinstance(scale, View):
            rd.append(scale)
            kw["scale"] = scale.ap
        else:
            kw["scale"] = scale
        return self.op("act", lambda g: g.activation(out.ap, in_.ap, func, **kw), rd, [out])

    def tt(self, out, a, b, op, e="dve"):
        return self.op(e, lambda g: g.tensor_tensor(out.ap, a.ap, b.ap, op), [a, b], [out])

    def ts(self, out, a, s1, op0, s2=None, op1=None, e="dve"):
        rd = [a]
        x1 = s1
        if isinstance(s1, View):
            rd.append(s1)
            x1 = s1.ap
        x2 = s2
        if isinstance(s2, View):
            rd.append(s2)
            x2 = s2.ap
        if op1 is None:
            return self.op(e, lambda g: g.tensor_scalar(out.ap, a.ap, x1, None, op0), rd, [out])
        return self.op(e, lambda g: g.tensor_scalar(out.ap, a.ap, x1, x2, op0, op1), rd, [out])

    def stt(self, out, a, s, b, op0, op1, e="dve"):
        rd = [a, b]
        x = s
        if isinstance(s, View):
            rd.append(s)
            x = s.ap
        return self.op(e, lambda g: g.scalar_tensor_tensor(out.ap, a.ap, x, b.ap, op0, op1), rd, [out])

    def copy(self, out, in_, e="dve"):
        if e == "act":
            return self.op("act", lambda g: g.copy(out.ap, in_.ap), [in_], [out])
        return self.op(e, lambda g: g.tensor_copy(out.ap, in_.ap), [in_], [out])

    def memset(self, out, val, e="dve"):
        return self.op(e, lambda g: g.memset(out.ap, val), [], [out])

    def reduce(self, out, in_, op, axis=AX.X, e="dve"):
        return self.op(e, lambda g: g.tensor_reduce(out.ap, in_.ap, axis, op), [in_], [out])

    def scan(self, out, d0, d1, init, op0=ALU.mult, op1=ALU.add, e="dve"):
        rd = [d0, d1]
        x = init
        if isinstance(init, View):
            rd.append(init)
            x = init.ap
        return self.op(e, lambda g: g.tensor_tensor_scan(out.ap, d0.ap, d1.ap, x, op0, op1), rd, [out])

    def recip(self, out, in_):
        return self.op("dve", lambda g: g.reciprocal(out.ap, in_.ap), [in_], [out])


D = 2048
KC = 16
NE = 32
DE = 768
ALPHA = 8.0 ** 0.25
LN_EPS = 1e-5
GN_EPS = 64e-5
NTOK = 2112
TILES = [(0, 512, 0), (512, 512, 0), (1024, 512, 0), (1536, 512, 0), (2048, 64, 1)]


def fm(t):
    return t.raw(t.t.ap().rearrange("(kc p) t -> p kc t", p=128))


def layer_norm_fm(p, X, N, onesS, PS_a, PS_b, sq, rstd, g_col, b_col):
    for kc in range(KC):
        p.mm(PS_a[:, :N], onesS[:], X[:, kc, :N], start=(kc == 0), stop=(kc == KC - 1))
    for kc in range(KC):
        p.tt(X[:, kc, :N], X[:, kc, :N], PS_a[:, :N], ALU.subtract)
    for kc in range(KC):
        s = sq[kc % 2]
        p.act(s[:, :N], X[:, kc, :N], AF.Square)
        p.mm(PS_b[:, :N], onesS[:], s[:, :N], start=(kc == 0), stop=(kc == KC - 1))
    p.act(rstd[:, :N], PS_b[:, :N], AF.Sqrt, bias=p.eps_ln[:, 0:1], scale=1.0)
    p.recip(rstd[:, :N], rstd[:, :N])
    for kc in range(KC):
        p.tt(X[:, kc, :N], X[:, kc, :N], rstd[:, :N], ALU.mult)
        p.act(X[:, kc, :N], X[:, kc, :N], AF.Identity, bias=b_col(kc), scale=g_col(kc))


def build_lb(kind, dbg=None):
    nc = bass.Bass("TRN2", target_bir_lowering=False)
    p = Prog(nc)
    xT = p.dram("xT", [D, NTOK], F32, kind="ExternalInput")
    if kind == "rwkv":
        ofT = p.dram("ofT", [D, NTOK], F32, kind="ExternalInput")
        obT = p.dram("obT", [D, NTOK], F32, kind="ExternalInput")
        ggT = p.dram("ggT", [D, NTOK], F32, kind="ExternalInput")
        bnT = p.dram("bnT", [D, NTOK], F32, kind="ExternalInput")
        lnx = p.dram("lnx", [128, 2, KC], F32, kind="ExternalInput")
    else:
        yT = p.dram("yT", [D, NTOK], F32, kind="ExternalInput")
    wo = p.dram("wo", [D, D], F32, kind="ExternalInput")
    bo = p.dram("bo", [128, KC], F32, kind="ExternalInput")
    mod = p.dram("mod", [128, 96, 2], F32, kind="ExternalInput")
    lngb = p.dram("lngb", [128, 4, KC], F32, kind="ExternalInput")
    rw = p.dram("rw", [D, NE], F32, kind="ExternalInput")
    rb = p.dram("rb", [NE], F32, kind="ExternalInput")
    NEd = 1 if dbg else NE
    w_in = p.dram("w_in", [NEd, D, 2 * DE], F32, kind="ExternalInput")
    b_in = p.dram("b_in", [128, NE, 12], F32, kind="ExternalInput")
    w_out = p.dram("w_out", [NEd, DE, D], F32, kind="ExternalInput")
    b_out = p.dram("b_out", [NE, D], F32, kind="ExternalInput")
    ident_d = p.dram("ident", [128, 128], F32, kind="ExternalInput")
    xo = p.dram("xo", [D, NTOK], F32, kind="ExternalOutput")

    X = p.sb([128, KC, 512], F32, "X")
    YH = p.sb([128, KC, 512], BF16, "YH")
    ACC = p.sb([128, KC, 512], F32, "ACC")
    ACTB = [p.sb([128, 6, 512], BF16, f"actb{i}") for i in range(2)]
    SW = [[p.sb([128, 512], F32, f"sw{i}{j}") for j in range(3)] for i in range(2)]
    WIN = [p.sb([128, KC, 512], BF16, f"win{i}") for i in range(2)]
    WOUT = [p.sb([128, 6, 1024], BF16, f"wout{i}") for i in range(2)]
    sq = [p.sb([128, 512], F32, f"sq{i}") for i in range(2)]
    rstd = p.sb([128, 512], F32, "rstd")
    tmpE = [p.sb([128, 512], F32, f"tmpE{i}") for i in range(2)]
    modS = p.sb([128, 96, 2], F32, "modS")
    sc4 = p.sb([128, KC, 2], F32, "sc4")
    lnS = p.sb([128, 4, KC], F32, "lnS")
    boS = p.sb([128, KC], F32, "boS")
    rwS = p.sb([128, KC, NE], F32, "rwS")
    rbS = p.sb([128, NE], F32, "rbS")
    binS = p.sb([128, NE, 12], F32, "binS")
    boutS = p.sb([NE, D], F32, "boutS")
    ident = p.sb([128, 128], F32, "ident")
    onesS = p.sb([128, 128], F32, "onesS")
    ones32 = p.sb([NE, 128], F32, "ones32")
    p.eps_ln = p.sb([128, 2], F32, "eps")
    GT = p.sb([NE, 512], F32, "GT")
    GE = [p.sb([NE, 512], F32, f"GE{i}") for i in range(2)]
    lg = p.sb([128, NE], F32, "lg")
    ex = p.sb([128, NE], F32, "ex")
    msk = p.sb([128, NE], F32, "msk")
    top8 = p.sb([128, 8], F32, "top8")
    sm = p.sb([128, 4], F32, "sm")
    if kind == "rwkv":
        lnxS = p.sb([128, 2, KC], F32, "lnxS")
        BD = p.sb([128, 128], F32, "BD")
        RI = [[p.sb([128, 512], F32, f"ri{i}{j}") for j in range(4)] for i in range(2)]
    else:
        YI = [p.sb([128, 512], F32, f"yi{i}") for i in range(2)]
    PS = [p.ps([128, 512], F32, f"ps{i}") for i in range(8)]

    p.dma("sp", modS[:], mod[:])
    p.dma("sp", lnS[:], lngb[:])
    p.dma("sp", boS[:], bo[:])
    p.dma("sp", rwS[:], rw.raw(rw.t.ap().rearrange("(kc p) e -> p kc e", p=128)))
    p.dma("sp", rbS[:], rb.raw(rb.t.ap().partition_broadcast(128)))
    p.dma("sp", binS[:], b_in[:])
    p.dma("sp", boutS[:], b_out[:])
    p.dma("sp", ident[:], ident_d[:])
    p.memset(onesS[:], 1.0 / D)
    p.memset(ones32[:], 1.0)
    p.memset(p.eps_ln[:, 0:1], LN_EPS)
    p.memset(p.eps_ln[:, 1:2], GN_EPS)
    p.ts(sc4[:], modS[:, 64:80, :], 1.0, ALU.add)
    if kind == "rwkv":
        p.dma("sp", lnxS[:], lnx[:])
        p.memset(BD[:], 0.0)
        p.memset(BD[0:64, 0:64], 1.0 / 64)
        p.memset(BD[64:128, 64:128], 1.0 / 64)

    xTv, xov = fm(xT), fm(xo)
    wo_v = wo.raw(wo.t.ap().rearrange("(kc p) c -> p kc c", p=128))

    for (t0, N, jm) in TILES:
        p.dma("sp", X[:, :, :N], xTv[:, :, t0:t0 + N])
        if kind == "rwkv":
            srcs = [fm(ofT), fm(obT), fm(ggT), fm(bnT)]
            for kc in range(KC):
                r = RI[kc % 2]
                for j in range(4):
                    p.dma("sp", r[j][:, :N], srcs[j][:, kc, t0:t0 + N])
                o, ob_, g_, bn_ = r
                p.tt(o[:, :N], o[:, :N], ob_[:, :N], ALU.add)
                p.mm(PS[0][:, :N], BD[:], o[:, :N])
                p.tt(o[:, :N], o[:, :N], PS[0][:, :N], ALU.subtract)
                p.act(ob_[:, :N], o[:, :N], AF.Square)
                p.mm(PS[1][:, :N], BD[:], ob_[:, :N])
                p.act(ob_[:, :N], PS[1][:, :N], AF.Sqrt, bias=p.eps_ln[:, 1:2], scale=1.0)
                p.recip(ob_[:, :N], ob_[:, :N])
                p.tt(o[:, :N], o[:, :N], ob_[:, :N], ALU.mult)
                p.act(o[:, :N], o[:, :N], AF.Identity, bias=lnxS[:, 1, kc:kc + 1], scale=lnxS[:, 0, kc:kc + 1])
                p.tt(o[:, :N], o[:, :N], bn_[:, :N], ALU.add)
                p.tt(YH[:, kc, :N], o[:, :N], g_[:, :N], ALU.mult)
        else:
            yTv = fm(yT)
            for kc in range(KC):
                yi = YI[kc % 2]
                p.dma("sp", yi[:, :N], yTv[:, kc, t0:t0 + N])
                p.copy(YH[:, kc, :N], yi[:, :N])
        for j in range(4):
            wb = WIN[j % 2]
            p.dma("pool", wb[:], wo_v[:, :, j * 512:(j + 1) * 512])
            for fl in range(4):
                fc = j * 4 + fl
                ps = PS[fc % 2]
                for kc in range(KC):
                    p.mmg(ps[:, :N], wb[:, kc, fl * 128:(fl + 1) * 128], YH[:, kc, :N], start=(kc == 0), stop=(kc == KC - 1))
                te = tmpE[fc % 2]
                p.ts(te[:, :N], ps[:, :N], boS[:, fc:fc + 1], ALU.add, modS[:, 32 + fc, jm:jm + 1], ALU.mult)
                p.stt(X[:, fc, :N], X[:, fc, :N], ALPHA, te[:, :N], ALU.mult, ALU.add)
        layer_norm_fm(p, X, N, onesS, PS[2], PS[3], sq, rstd,
                      lambda kc: lnS[:, 0, kc:kc + 1], lambda kc: lnS[:, 1, kc:kc + 1])
        if dbg == "x1":
            p.dma("sp", xov[:, :, t0:t0 + N], X[:, :, :N])
            continue
        for kc in range(KC):
            p.act(ACC[:, kc, :N], X[:, kc, :N], AF.Identity, bias=modS[:, 48 + kc, jm:jm + 1], scale=sc4[:, kc, jm:jm + 1])
            p.copy(YH[:, kc, :N], ACC[:, kc, :N])
        nsub = (N + 127) // 128
        for s in range(nsub):
            n = min(128, N - s * 128)
            pl = PS[4 + (s % 2)]
            for kc in range(KC):
                p.mmg(pl[:n, 0:NE], ACC[:, kc, s * 128:s * 128 + n], rwS[:, kc, :], start=(kc == 0), stop=(kc == KC - 1))
            p.tt(lg[:n, :], pl[:n, 0:NE], rbS[:n, :], ALU.add)
            p.op("dve", lambda g, n=n: g.max(top8[:n, :].ap, lg[:n, :].ap), [lg[:n, :]], [top8[:n, :]])
            p.ts(sm[:n, 0:1], top8[:n, 0:1], -1.0, ALU.mult)
            p.act(ex[:n, :], lg[:n, :], AF.Exp, bias=sm[:n, 0:1], scale=1.0)
            p.ts(msk[:n, :], lg[:n, :], top8[:n, 3:4], ALU.is_ge)
            p.tt(ex[:n, :], ex[:n, :], msk[:n, :], ALU.mult)
            p.reduce(sm[:n, 1:2], ex[:n, :], ALU.add)
            p.recip(sm[:n, 2:3], sm[:n, 1:2])
            p.ts(ex[:n, :], ex[:n, :], sm[:n, 2:3], ALU.mult)
            pt = PS[6 + (s % 2)]
            p.tr(pt[0:NE, 0:n], ex[:n, :], ident[:n, :n])
            p.copy(GT[:, s * 128:s * 128 + n], pt[0:NE, 0:n])
        def emit_in(e):
            ab = ACTB[e % 2]
            ge = GE[e % 2]
            p.ts(ge[:, :N], GT[:, :N], ident[0:NE, e:e + 1], ALU.mult)
            for j in range(3):
                wb = WIN[(e * 3 + j) % 2]
                srcv = w_in.t.ap()[e].rearrange("(kc p) c -> p kc c", p=128)
                for h in range(2):
                    p.dma("pool", wb[:, :, h * 256:(h + 1) * 256],
                          w_in.raw(srcv[:, :, h * 768 + j * 256:h * 768 + (j + 1) * 256]))
                for hl in range(2):
                    hc = j * 2 + hl
                    pg, pu = PS[(hc % 2) * 2], PS[(hc % 2) * 2 + 1]
                    for kc in range(KC):
                        p.mmg(pg[:, :N], wb[:, kc, hl * 128:(hl + 1) * 128], YH[:, kc, :N], start=(kc == 0), stop=(kc == KC - 1))
                    for kc in range(KC):
                        p.mmg(pu[:, :N], wb[:, kc, 256 + hl * 128:256 + (hl + 1) * 128], YH[:, kc, :N], start=(kc == 0), stop=(kc == KC - 1))
                    if hc == 0:
                        p.mm(PS[4 + 3 * (e % 2)][:, :N], ones32[:], ge[:, :N])
                    g_, s_, u_ = SW[hc % 2]
                    p.ts(g_[:, :N], pg[:, :N], binS[:, e, hc:hc + 1], ALU.add, 7.0, ALU.min)
                    p.act(s_[:, :N], g_[:, :N], AF.Sigmoid, scale=1.702)
                    p.ts(u_[:, :N], pu[:, :N], binS[:, e, 6 + hc:7 + hc], ALU.add, 7.0, ALU.min)
                    p.ts(u_[:, :N], u_[:, :N], -7.0, ALU.max, 1.0, ALU.add)
                    p.tt(g_[:, :N], g_[:, :N], s_[:, :N], ALU.mult)
                    p.tt(g_[:, :N], g_[:, :N], u_[:, :N], ALU.mult)
                    p.tt(ab[:, hc, :N], g_[:, :N], PS[4 + 3 * (e % 2)][:, :N], ALU.mult)

        def emit_out(e):
            ab = ACTB[e % 2]
            for h in range(2):
                wb = WOUT[(e * 2 + h) % 2]
                src = w_out.raw(w_out.t.ap()[e].rearrange("(hc p) c -> p hc c", p=128)[:, :, h * 1024:(h + 1) * 1024])
                p.dma("pool", wb[:], src)
                for fl in range(8):
                    fc = h * 8 + fl
                    po = PS[5 + (fc % 2)]
                    first = True
                    if e == 0:
                        p.mmg(po[:, :N], boutS[:, fc * 128:(fc + 1) * 128], GT[:, :N], start=True, stop=False)
                        first = False
                    for hc in range(6):
                        p.mmg(po[:, :N], wb[:, hc, fl * 128:(fl + 1) * 128], ab[:, hc, :N], start=(first and hc == 0), stop=(hc == 5))
                    if e == 0:
                        p.copy(ACC[:, fc, :N], po[:, :N])
                    else:
                        p.tt(ACC[:, fc, :N], ACC[:, fc, :N], po[:, :N], ALU.add)

        emit_in(0)
        for e in range(NE):
            if e + 1 < NE:
                emit_in(e + 1)
            emit_out(e)
        for kc in range(KC):
            te = tmpE[kc % 2]
            p.ts(te[:, :N], ACC[:, kc, :N], modS[:, 80 + kc, jm:jm + 1], ALU.mult)
            p.stt(X[:, kc, :N], X[:, kc, :N], ALPHA, te[:, :N], ALU.mult, ALU.add)
        layer_norm_fm(p, X, N, onesS, PS[2], PS[3], sq, rstd,
                      lambda kc: lnS[:, 2, kc:kc + 1], lambda kc: lnS[:, 3, kc:kc + 1])
        p.dma("sp", xov[:, :, t0:t0 + N], X[:, :, :N])
    p.wait_all("sp", [xo[:]])
    print("LB", kind, "instr", p.ninst, "sems", p.nsem, "sbuf left", nc.sbuf_bytes_remaining)
    return nc


T_ALL = 8448
LRU_TILES = [(0, 256, 1)] + [(256 + 512 * i, 512, 0) for i in range(16)]
GELU_C = 1.5957691216057308


def rcol(s0, jm):
    return s0 + 2 if jm == 1 else s0 + 5


def build_lru():
    nc = bass.Bass("TRN2", target_bir_lowering=False)
    p = Prog(nc)
    T = T_ALL
    xT = p.dram("xT", [D, T], F32, kind="ExternalInput")
    mod = p.dram("mod", [128, 96, 2], F32, kind="ExternalInput")
    w = p.dram("w", [D, 1024], F32, kind="ExternalInput")
    convw = p.dram("convw", [128, 4, 4], F32, kind="ExternalInput")
    convb = p.dram("convb", [128, 4], F32, kind="ExternalInput")
    gw = p.dram("gw", [2, 2, 2, 256, 256], F32, kind="ExternalInput")
    gb = p.dram("gb", [128, 2, 2, 4], F32, kind="ExternalInput")
    lam = p.dram("lam", [128, 2, 4], F32, kind="ExternalInput")
    yl = p.dram("yl", [512, T], F32, kind="ExternalOutput")
    Rsc = p.dram("Rsc", [512, T + 6], F32)
    Ysc = p.dram("Ysc", [512, T], F32)
    HF = p.dram("HF", [512, T], F32)
    A1 = p.dram("A1", [512, T], F32)
    U1 = p.dram("U1", [512, T], F32)

    def cm(t):
        return t.raw(t.t.ap().rearrange("(c p) t -> p c t", p=128))

    W = p.sb([128, KC, 1024], BF16, "W")
    GW = p.sb([128, 16, 256], BF16, "GW")
    modS = p.sb([128, 96, 2], F32, "modS")
    sc1 = p.sb([128, KC, 2], F32, "sc1")
    cwS = p.sb([128, 4, 4], F32, "cwS")
    cbS = p.sb([128, 4], F32, "cbS")
    gbS = p.sb([128, 2, 2, 4], F32, "gbS")
    lamS = p.sb([128, 2, 4], F32, "lamS")
    cz = p.sb([128, 2, 4], F32, "cz")
    zero = p.sb([128, 4, 3], F32, "zero")
    p_one = p.sb([128, 1], F32, "p_one")
    p.memset(p_one[:], 1.0)
    XI = [p.sb([128, 512], F32, f"xi{i}") for i in range(4)]
    H = [p.sb([128, KC, 512], BF16, f"H{i}") for i in range(1)]
    GT_ = [[p.sb([128, 512], F32, f"gt{i}{j}") for j in range(2)] for i in range(2)]
    YO = [p.sb([128, 512], F32, f"yo{i}") for i in range(2)]
    RO = [p.sb([128, 512], F32, f"ro{i}") for i in range(2)]
    PS = [p.ps([128, 512], F32, f"ps{i}") for i in range(8)]

    p.dma("sp", modS[:], mod[:])
    p.dma("sp", cwS[:], convw[:])
    p.dma("sp", cbS[:], convb[:])
    p.dma("sp", gbS[:], gb[:])
    p.dma("sp", lamS[:], lam[:])
    p.dma("pool", W[:], w.raw(w.t.ap().rearrange("(kc p) c -> p kc c", p=128)))
    p.dma("pool", GW[:], gw.raw(gw.t.ap().rearrange("z g b (kc p) m -> p (z g b kc) m", p=128)))
    p.ts(sc1[:], modS[:, 16:32, :], 1.0, ALU.add)
    p.act(cz[:], lamS[:], AF.Exp, scale=-1.0)
    p.act(cz[:], cz[:], AF.Ln, bias=p_one[:, 0:1], scale=1.0)
    p.ts(cz[:], cz[:], -8.0, ALU.mult)
    p.memset(zero[:], 0.0)
    Rv, Yv, HFv, A1v, U1v, ylv, xv = cm(Rsc), cm(Ysc), cm(HF), cm(A1), cm(U1), cm(yl), cm(xT)
    p.dma("sp", Rv[:, :, 0:2], zero[:, :, 0:2])
    p.dma("sp", Rv[:, :, 258:261], zero[:, :, 0:3])
    p.dma("sp", Rv[:, :, T + 5:T + 6], zero[:, :, 0:1], allow_slow_non_contiguous=True)

    for ti, (s0, N, jm) in enumerate(LRU_TILES):
        Hb = H[0]
        for kc in range(KC):
            xi = XI[kc % 4]
            p.dma("sp", xi[:, :N], xv[:, kc, s0:s0 + N])
            p.act(Hb[:, kc, :N], xi[:, :N], AF.Identity, bias=modS[:, kc, jm:jm + 1], scale=sc1[:, kc, jm:jm + 1])
        for mc in range(4):
            ps = PS[mc % 2]
            for kc in range(KC):
                p.mmg(ps[:, :N], W[:, kc, mc * 128:(mc + 1) * 128], Hb[:, kc, :N], start=(kc == 0), stop=(kc == KC - 1))
            t1, t2 = GT_[mc % 2]
            yo = YO[mc % 2]
            p.act(t1[:, :N], ps[:, :N], AF.Square)
            p.ts(t1[:, :N], t1[:, :N], 0.044715, ALU.mult, 1.0, ALU.add)
            p.tt(t1[:, :N], t1[:, :N], ps[:, :N], ALU.mult)
            p.act(t2[:, :N], t1[:, :N], AF.Sigmoid, scale=GELU_C)
            p.tt(yo[:, :N], t2[:, :N], ps[:, :N], ALU.mult)
            p.dma("sp", Yv[:, mc, s0:s0 + N], yo[:, :N])
        for mc in range(4):
            ps = PS[2 + mc % 2]
            for kc in range(KC):
                p.mmg(ps[:, :N], W[:, kc, 512 + mc * 128:512 + (mc + 1) * 128], Hb[:, kc, :N], start=(kc == 0), stop=(kc == KC - 1))
            ro = RO[mc % 2]
            p.copy(ro[:, :N], ps[:, :N], e="act")
            c0 = rcol(s0, jm)
            p.dma("sp", Rv[:, mc, c0:c0 + N], ro[:, :N])

    RH = [p.sb([128, 4, 515], F32, f"rh{i}") for i in range(2)]
    XC = [p.sb([128, 4, 512], F32, f"xc{i}") for i in range(2)]
    XCb = [p.sb([128, 4, 512], BF16, f"xcb{i}") for i in range(2)]
    G = [[p.sb([128, 4, 512], F32, f"g{z}{g}") for g in range(2)] for z in range(2)]
    AA = [p.sb([128, 4, 512], F32, f"aa{z}") for z in range(2)]
    UU = [p.sb([128, 4, 512], F32, f"uu{z}") for z in range(2)]
    HFs = [p.sb([128, 4, 512], F32, f"hfs{i}") for i in range(2)]
    carryf = p.sb([128, 4], F32, "carryf")
    carryb = p.sb([128, 4], F32, "carryb")
    p.memset(carryf[:], 0.0)
    p.memset(carryb[:], 0.0)
    for ti, (s0, N, jm) in enumerate(LRU_TILES):
        b = ti % 2
        rh, xc, xcb = RH[b], XC[b], XCb[b]
        c0 = rcol(s0, jm) - 2
        p.dma("sp", rh[:, :, :N + 3], Rv[:, :, c0:c0 + N + 3])
        for c in range(4):
            p.ts(xc[:, c, :N], rh[:, c, 0:N], cwS[:, c, 0:1], ALU.mult, cbS[:, c:c + 1], ALU.add)
            for j in range(1, 4):
                p.stt(xc[:, c, :N], rh[:, c, j:j + N], cwS[:, c, j:j + 1], xc[:, c, :N], ALU.mult, ALU.add)
            p.copy(xcb[:, c, :N], xc[:, c, :N], e="act")
        k = 0
        for z in range(2):
            for g in range(2):
                for blk in range(2):
                    for m2 in range(2):
                        ps = PS[4 + k % 4]
                        k += 1
                        for k2 in range(2):
                            p.mmg(ps[:, :N], GW[:, ((z * 2 + g) * 2 + blk) * 2 + k2, m2 * 128:(m2 + 1) * 128],
                                 xcb[:, blk * 2 + k2, :N], start=(k2 == 0), stop=(k2 == 1))
                        c = blk * 2 + m2
                        p.act(G[z][g][:, c, :N], ps[:, :N], AF.Sigmoid, bias=gbS[:, z, g, c:c + 1], scale=1.0)
        for z in range(2):
            a, u = AA[z], UU[z]
            for c in range(4):
                p.act(a[:, c, :N], G[z][0][:, c, :N], AF.Exp, scale=cz[:, z, c:c + 1])
            for c in range(4):
                p.tt(u[:, c, :N], a[:, c, :N], a[:, c, :N], ALU.mult)
                p.ts(u[:, c, :N], u[:, c, :N], -1.0, ALU.mult, 1.0, ALU.add)
            for c in range(4):
                p.act(u[:, c, :N], u[:, c, :N], AF.Sqrt)
            for c in range(4):
                p.tt(u[:, c, :N], u[:, c, :N], G[z][1][:, c, :N], ALU.mult)
                p.tt(u[:, c, :N], u[:, c, :N], xc[:, c, :N], ALU.mult)
        hf = HFs[b]
        for c in range(4):
            p.scan(hf[:, c, :N], AA[0][:, c, :N], UU[0][:, c, :N], carryf[:, c:c + 1])
            p.copy(carryf[:, c:c + 1], hf[:, c, N - 1:N])
        p.dma("sp", HFv[:, :, s0:s0 + N], hf[:, :, :N])
        p.dma("sp", A1v[:, :, s0:s0 + N], AA[1][:, :, :N])
        p.dma("sp", U1v[:, :, s0:s0 + N], UU[1][:, :, :N])

    def rev(v, n):
        a = v.ap
        pst = a.ap[0][0]
        return v.with_ap(bass.AP(a.tensor, a.offset + n - 1, [[pst, 128], [-1, n]]))

    for ti, (s0, N, jm) in [(0, LRU_TILES[0])] + list(enumerate(LRU_TILES))[:0:-1]:
        b = ti % 2
        a1, u1, hf, yy = AA[b], UU[b], HFs[b], XC[b]
        hb = G[b][0]
        p.dma("sp", a1[:, :, :N], A1v[:, :, s0:s0 + N])
        p.dma("sp", u1[:, :, :N], U1v[:, :, s0:s0 + N])
        p.dma("sp", hf[:, :, :N], HFv[:, :, s0:s0 + N])
        p.dma("sp", yy[:, :, :N], Yv[:, :, s0:s0 + N])
        for c in range(4):
            p.scan(rev(hb[:, c, :N], N), rev(a1[:, c, :N], N), rev(u1[:, c, :N], N), carryb[:, c:c + 1])
            p.copy(carryb[:, c:c + 1], hb[:, c, 0:1])
            p.tt(hb[:, c, :N], hb[:, c, :N], hf[:, c, :N], ALU.add)
            p.tt(hb[:, c, :N], hb[:, c, :N], yy[:, c, :N], ALU.mult)
        p.dma("sp", ylv[:, :, s0:s0 + N], hb[:, :, :N])
    p.wait_all("sp", [yl[:]])
    print("LRU instr", p.ninst, "sems", p.nsem, "sbuf left", nc.sbuf_bytes_remaining)
    return nc


HD = 64


def build_att():
    nc = bass.Bass("TRN2", target_bir_lowering=False)
    p = Prog(nc)
    T = T_ALL
    xT = p.dram("xT", [D, T], F32, kind="ExternalInput")
    mod = p.dram("mod", [128, 96, 2], F32, kind="ExternalInput")
    wq = p.dram("wq", [D, 1024], F32, kind="ExternalInput")
    wk = p.dram("wk", [D, 192], F32, kind="ExternalInput")
    bq = p.dram("bq", [HD, 16], F32, kind="ExternalInput")
    bk = p.dram("bk", [HD, 2], F32, kind="ExternalInput")
    bv = p.dram("bv", [HD], F32, kind="ExternalInput")
    cosT = p.dram("cosT", [HD, 8192], F32, kind="ExternalInput")
    sinT = p.dram("sinT", [HD, 8192], F32, kind="ExternalInput")
    sink = p.dram("sink", [8], F32, kind="ExternalInput")
    masks = p.dram("masks", [128, 2, 128], F32, kind="ExternalInput")
    o = p.dram("o", [T, 512], F32, kind="ExternalOutput")

    def cm(t):
        return t.raw(t.t.ap().rearrange("(c p) t -> p c t", p=128))

    WQ = p.sb([128, KC, 1024], BF16, "WQ")
    WK = p.sb([128, KC, 192], BF16, "WK")
    modS = p.sb([128, 96, 2], F32, "modS")
    sc1 = p.sb([128, KC, 2], F32, "sc1")
    bqS = p.sb([HD, 16], F32, "bqS")
    bkS = p.sb([HD, 2], F32, "bkS")
    bvS = p.sb([128, HD], F32, "bvS")
    snk = p.sb([128, 8], F32, "snk")
    mk = p.sb([128, 2, 128], BF16, "mk")
    KT = p.sb([HD, T], BF16, "KT")
    V = p.sb([128, 66, 65], BF16, "V")
    XI = [p.sb([128, 512], F32, f"xi{i}") for i in range(4)]
    H = p.sb([128, KC, 512], BF16, "H")
    CS = [p.sb([HD, 2, 512], F32, f"cs{i}") for i in range(2)]
    QT = p.sb([HD, 8, 512], BF16, "QT")
    t64 = [[p.sb([HD, 512], F32, f"t64{i}{j}") for j in range(2)] for i in range(2)]
    PB = [p.sb([128, 512], BF16, f"pb{i}") for i in range(6)]
    OB = [p.sb([128, 512], F32, f"ob{i}") for i in range(2)]
    den = [p.sb([128, 4], F32, f"den{i}") for i in range(2)]
    PS = [p.ps([128, 512], F32, f"ps{i}") for i in range(8)]

    p.dma("sp", modS[:], mod[:])
    p.dma("sp", bqS[:], bq[:])
    p.dma("sp", bkS[:], bk[:])
    p.dma("sp", bvS[:], bv.raw(bv.t.ap().partition_broadcast(128)))
    p.dma("sp", snk[:], sink.raw(sink.t.ap().partition_broadcast(128)))
    p.dma("pool", mk[:], masks[:])
    p.dma("pool", WQ[:], wq.raw(wq.t.ap().rearrange("(kc p) c -> p kc c", p=128)))
    p.dma("pool", WK[:], wk.raw(wk.t.ap().rearrange("(kc p) c -> p kc c", p=128)))
    p.ts(sc1[:], modS[:, 16:32, :], 1.0, ALU.add)
    p.act(snk[:], snk[:], AF.Exp)
    p.memset(V[:, :, 64:65], 1.0)
    xv = cm(xT)

    def load_h(s0, N, jm):
        for kc in range(KC):
            xi = XI[kc % 4]
            p.dma("sp", xi[:, :N], xv[:, kc, s0:s0 + N])
            p.act(H[:, kc, :N], xi[:, :N], AF.Identity, bias=modS[:, kc, jm:jm + 1], scale=sc1[:, kc, jm:jm + 1])

    def load_cs(ti, s0, N):
        cs = CS[ti % 2]
        p.dma("sp", cs[:, 0, :N], cosT[:, s0 - 256:s0 - 256 + N])
        p.dma("sp", cs[:, 1, :N], sinT[:, s0 - 256:s0 - 256 + N])
        return cs

    def proj_rope(dst, wtile, c_plain, c_perm, b_plain, b_perm, N, lat, cs, scr):
        pa, pb = PS[0], PS[1]
        for kc in range(KC):
            p.mmg(pa[0:HD, :N], wtile[:, kc, c_plain:c_plain + HD], H[:, kc, :N], start=(kc == 0), stop=(kc == KC - 1))
        if not lat:
            p.ts(dst, pa[0:HD, :N], b_plain, ALU.add)
            return
        for kc in range(KC):
            p.mmg(pb[0:HD, :N], wtile[:, kc, c_perm:c_perm + HD], H[:, kc, :N], start=(kc == 0), stop=(kc == KC - 1))
        ta, tb = scr
        p.stt(ta[:, :N], pa[0:HD, :N], b_plain, cs[:, 0, :N], ALU.add, ALU.mult)
        p.stt(tb[:, :N], pb[0:HD, :N], b_perm, cs[:, 1, :N], ALU.add, ALU.mult)
        p.tt(dst, ta[:, :N], tb[:, :N], ALU.add)

    for ti, (s0, N, jm) in enumerate(LRU_TILES):
        lat = jm == 0
        load_h(s0, N, jm)
        cs = load_cs(ti, s0, N) if lat else None
        proj_rope(KT[:, s0:s0 + N], WK, 0, 64, bkS[:, 0:1], bkS[:, 1:2], N, lat, cs, t64[ti % 2])
        for s in range(N // 128):
            pv = PS[2 + s % 2]
            for kc in range(KC):
                p.mmg(pv[:, 0:HD], H[:, kc, s * 128:(s + 1) * 128], WK[:, kc, 128:192], start=(kc == 0), stop=(kc == KC - 1))
            p.tt(V[:, (s0 // 128) + s, 0:HD], pv[:, 0:HD], bvS[:], ALU.add)

    ov = o
    for ti, (s0, N, jm) in enumerate(LRU_TILES):
        lat = jm == 0
        load_h(s0, N, jm)
        cs = load_cs(ti, s0, N) if lat else None
        for h in range(8):
            proj_rope(QT[:, h, :N], WQ, h * HD, 512 + h * HD, bqS[:, h:h + 1], bqS[:, 8 + h:9 + h], N, lat, cs, t64[h % 2])
        for qb in range(N // 128):
            kt0 = s0 // 128 + qb
            if lat:
                bi = kt0 - 2
                keys = ([(kt0 - 1, 0)] if bi > 0 else []) + [(kt0, None)] + ([(kt0 + 1, 1)] if bi < 63 else []) + [(0, None), (1, None)]
            else:
                keys = [(0, None), (1, None)]
            ob = OB[qb % 2]
            for hg in range(2):
                pbs = []
                for ki, (kt, mtype) in enumerate(keys):
                    pss = PS[2 + ki % 2]
                    p.mm(pss.raw(pss.t[:].rearrange("p (h q) -> p h q", h=4)), KT[:, kt * 128:(kt + 1) * 128], QT[:, hg * 4:(hg + 1) * 4, qb * 128:(qb + 1) * 128])
                    pb_ = PB[ki]
                    p.act(pb_[:], pss[:], AF.Exp, scale=0.125)
                    if mtype is not None:
                        v3 = pb_.raw(pb_.t[:].rearrange("p (h q) -> p h q", h=4))
                        m3 = mk.raw(mk.t[:, mtype:mtype + 1, :].to_broadcast([128, 4, 128]))
                        p.tt(v3, v3, m3, ALU.mult, e="pool")
                    pbs.append(pb_)
                po = PS[4 + hg]
                for h in range(4):
                    for ki, (kt, mtype) in enumerate(keys):
                        p.mmg(po[:, h * 65:(h + 1) * 65], pbs[ki][:, h * 128:(h + 1) * 128], V[:, kt, :],
                              start=(ki == 0), stop=(ki == len(keys) - 1))
                dn = den[hg]
                po3 = po.raw(po.t[:, 0:260].rearrange("p (h c) -> p h c", h=4))
                p.tt(dn[:], po3[:, :, 64], snk[:, hg * 4:(hg + 1) * 4], ALU.add)
                p.recip(dn[:], dn[:])
                for h in range(4):
                    p.ts(ob[:, (hg * 4 + h) * 64:(hg * 4 + h + 1) * 64], po[:, h * 65:h * 65 + 64], dn[:, h:h + 1], ALU.mult)
            r0 = s0 + qb * 128
            p.dma("sp", ov[r0:r0 + 128, :], ob[:])
    p.wait_all("sp", [o[:]])
    print("ATT instr", p.ninst, "sems", p.nsem, "sbuf left", nc.sbuf_bytes_remaining)
    return nc


NH = 16
TC = 4
DEC = -0.6065306597126334


def build_rwkv(has_vres):
    nc = bass.Bass("TRN2", target_bir_lowering=False)
    p = Prog(nc)
    T = T_ALL
    NL = 608 if has_vres else 544
    xT = p.dram("xT", [D, T], F32, kind="ExternalInput")
    mod = p.dram("mod", [128, 96, 2], F32, kind="ExternalInput")
    mix = p.dram("mix", [128, KC, 6], F32, kind="ExternalInput")
    wrkv = p.dram("wrkv", [2, D, 1536], F32, kind="ExternalInput")
    wl1 = p.dram("wl1", [D, NL], F32, kind="ExternalInput")
    w2 = p.dram("w2", [96, 1024], F32, kind="ExternalInput")
    a2 = p.dram("a2", [2, 96, 1024], F32, kind="ExternalInput")
    g2 = p.dram("g2", [256, 1024], F32, kind="ExternalInput")
    vecs = p.dram("vecs", [7, 1024], F32, kind="ExternalInput")
    if has_vres:
        v2 = p.dram("v2", [64, 1024], F32, kind="ExternalInput")
        vf = p.dram("vf", [T, 1024], F32, kind="ExternalInput")
    O2 = p.dram("O2", [128, T, 8], F32, kind="ExternalOutput")
    gout = p.dram("gout", [T, 1024], F32, kind="ExternalOutput")
    bout = p.dram("bout", [T, 1024], F32, kind="ExternalOutput")
    vout = p.dram("vout", [T, 1024], F32, kind="ExternalOutput")
    SX = [p.dram(f"S{n}", [NH, T, 64], F32) for n in "rwkab"]
    Sv2 = p.dram("Sv2", [128, T, 8], F32)

    WR = p.sb([128, KC, 1536], BF16, "WR")
    WL = p.sb([128, KC, NL], BF16, "WL")
    W2 = p.sb([96, 512], BF16, "W2")
    A2 = p.sb([96, 2, 512], BF16, "A2")
    G2 = p.sb([128, 2, 512], BF16, "G2")
    VEC = p.sb([128, 7, 512], F32, "VEC")
    modS = p.sb([128, 96, 2], F32, "modS")
    sc1 = p.sb([128, KC, 2], F32, "sc1")
    mixS = p.sb([128, KC, 6], F32, "mixS")
    Hh = p.sb([128, KC, 130], F32, "Hh")
    XX = p.sb([128, KC, 128], F32, "XX")
    XM = [p.sb([128, KC, 128], BF16, f"xm{m}") for m in range(6)]
    LW = p.sb([96, 128], BF16, "LW")
    LA = p.sb([96, 2, 128], BF16, "LA")
    LG = p.sb([128, 2, 128], BF16, "LG")
    if has_vres:
        V2 = p.sb([64, 512], BF16, "V2")
        LV = p.sb([64, 128], BF16, "LV")
        VF = p.sb([128, 512], F32, "VF")
    names = ["R", "K", "V", "Wd", "Io", "Iot", "KK", "t1", "t2", "Kd", "Aa", "Bb", "G", "BN"]
    Wk = {n: p.sb([128, 512], F32, "wk" + n) for n in names}
    small = p.sb([128, 4, 8], F32, "small")
    PS = [p.ps([128, 512], F32, f"ps{i}") for i in range(8)]

    p.dma("sp", modS[:], mod[:])
    p.dma("sp", mixS[:], mix[:])
    p.dma("pool", WL[:], wl1.raw(wl1.t.ap().rearrange("(kc p) c -> p kc c", p=128)))
    p.ts(sc1[:], modS[:, 16:32, :], 1.0, ALU.add)
    xv = xT.raw(xT.t.ap().rearrange("(kc p) t -> p kc t", p=128))

    def h3(t):
        return t.raw(t.t[:].rearrange("p (h n) -> p h n", h=8))

    for half in range(2):
        c0 = half * 512
        p.dma("pool", WR[:], wrkv.raw(wrkv.t.ap()[half].rearrange("(kc p) c -> p kc c", p=128)))
        p.dma("pool", W2[:], w2[:, c0:c0 + 512])
        p.dma("pool", A2[:], a2.raw(a2.t.ap().rearrange("z r c -> r z c")[:, :, c0:c0 + 512]))
        p.dma("pool", G2[:], g2.raw(g2.t.ap().rearrange("(k p) c -> p k c", p=128)[:, :, c0:c0 + 512]))
        for i in range(7):
            p.dma("sp", VEC[:, i, :], vecs.raw(vecs.t.ap()[i, c0:c0 + 512].partition_broadcast(128)))
        if has_vres:
            p.dma("pool", V2[:], v2[:, c0:c0 + 512])
        for ti in range(66):
            s0 = ti * 128
            jm = 1 if ti < 2 else 0
            left_edge = ti in (0, 2)
            right_edge = ti in (1, 65)
            lo = 1 if left_edge else 0
            hi = 129 if right_edge else 130
            p.dma("sp", Hh[:, :, lo:hi], xv[:, :, s0 - 1 + lo:s0 - 1 + hi])
            for kc in range(KC):
                p.act(Hh[:, kc, lo:hi], Hh[:, kc, lo:hi], AF.Identity, bias=modS[:, kc, jm:jm + 1], scale=sc1[:, kc, jm:jm + 1])
            if left_edge:
                p.memset(Hh[:, :, 0:1], 0.0)
            if right_edge:
                p.memset(Hh[:, :, 129:130], 0.0)
            p.tt(XX[:], Hh[:, :, 0:128], Hh[:, :, 2:130], ALU.add)
            p.stt(XX[:], XX[:], 0.5, Hh[:, :, 1:129], ALU.mult, ALU.subtract)
            for m in range(6):
                for kc in range(KC):
                    p.stt(XM[m][:, kc, :], XX[:, kc, :], mixS[:, kc, m:m + 1], Hh[:, kc, 1:129], ALU.mult, ALU.add)
            xr, xw, xk, xv_, xa, xg = XM
            def lora1(ps, col, width, src):
                for kc in range(KC):
                    p.mmg(ps[0:width, 0:128], WL[:, kc, col:col + width], src[:, kc, :], start=(kc == 0), stop=(kc == KC - 1))
            lora1(PS[0], 0, 96, xw)
            p.act(LW[:], PS[0][0:96, 0:128], AF.Tanh)
            lora1(PS[1], 96, 96, xa)
            p.copy(LA[:, 0, :], PS[1][0:96, 0:128])
            lora1(PS[2], 192, 96, xa)
            p.copy(LA[:, 1, :], PS[2][0:96, 0:128])
            for k2 in range(2):
                lora1(PS[3 + k2], 288 + k2 * 128, 128, xg)
                p.act(LG[:, k2, :], PS[3 + k2][:, 0:128], AF.Sigmoid)
            if has_vres:
                lora1(PS[5], 544, 64, xv_)
                p.copy(LV[:], PS[5][0:64, 0:128])
            for j, src in enumerate((xr, xk, xv_)):
                for kc in range(KC):
                    p.mmg(PS[j][:], src[:, kc, :], WR[:, kc, j * 512:(j + 1) * 512], start=(kc == 0), stop=(kc == KC - 1))
            R, K, V = Wk["R"], Wk["K"], Wk["V"]
            p.copy(R[:], PS[0][:], e="act")
            p.copy(K[:], PS[1][:], e="act")
            p.mm(PS[3][:], LW[:], W2[:])
            p.mm(PS[4][:], LA[:, 0, :], A2[:, 0, :])
            p.mm(PS[5][:], LA[:, 1, :], A2[:, 1, :])
            for k2 in range(2):
                p.mm(PS[6][:], LG[:, k2, :], G2[:, k2, :], start=(k2 == 0), stop=(k2 == 1))
            if has_vres:
                p.mm(PS[7][:], LV[:], V2[:])
                p.dma("sp", VF[:], vf[s0:s0 + 128, c0:c0 + 512])
                t1, t2 = Wk["t1"], Wk["t2"]
                p.tt(t1[:], PS[7][:], VEC[:, 6, :], ALU.add)
                p.act(t1[:], t1[:], AF.Sigmoid)
                p.tt(t2[:], VF[:], PS[2][:], ALU.subtract)
                p.tt(t2[:], t2[:], t1[:], ALU.mult)
                p.tt(V[:], t2[:], PS[2][:], ALU.add)
            else:
                p.copy(V[:], PS[2][:], e="act")
            Wd, Io, Iot, KK, t1, t2 = Wk["Wd"], Wk["Io"], Wk["Iot"], Wk["KK"], Wk["t1"], Wk["t2"]
            Kd, Aa, Bb, G, BN = Wk["Kd"], Wk["Aa"], Wk["Bb"], Wk["G"], Wk["BN"]
            p.tt(Wd[:], PS[3][:], VEC[:, 0, :], ALU.add)
            p.act(Wd[:], Wd[:], AF.Sigmoid)
            p.act(Wd[:], Wd[:], AF.Exp, scale=DEC)
            p.tt(Io[:], PS[4][:], VEC[:, 1, :], ALU.add)
            p.act(Io[:], Io[:], AF.Sigmoid)
            p.tt(Iot[:], PS[5][:], VEC[:, 2, :], ALU.add)
            p.act(Iot[:], Iot[:], AF.Sigmoid)
            p.copy(G[:], PS[6][:], e="act")
            p.tt(KK[:], K[:], VEC[:, 3, :], ALU.mult)
            p.tt(t1[:], KK[:], KK[:], ALU.mult)
            p.reduce(small[:, 0, :], h3(t1), ALU.add)
            p.act(small[:, 0, :], small[:, 0, :], AF.Sqrt)
            p.ts(small[:, 0, :], small[:, 0, :], 1e-12, ALU.max)
            p.recip(small[:, 1, :], small[:, 0, :])
            p.tt(h3(KK), h3(KK), small.raw(small.t[:, 1, :].unsqueeze(2).to_broadcast([128, 8, 64])), ALU.mult)
            p.stt(t1[:], Io[:], -1.0, VEC[:, 4, :], ALU.add, ALU.mult)
            p.ts(t1[:], t1[:], 1.0, ALU.add)
            p.tt(Kd[:], K[:], t1[:], ALU.mult)
            p.ts(Aa[:], KK[:], -1.0, ALU.mult)
            p.tt(Bb[:], KK[:], Io[:], ALU.mult)
            p.tt(t2[:], Io[:], Iot[:], ALU.add)
            p.ts(t2[:], t2[:], 0.5, ALU.mult, -1.0, ALU.add)
            p.tt(t2[:], t2[:], VEC[:, 4, :], ALU.mult)
            p.ts(t2[:], t2[:], 1.0, ALU.add)
            p.tt(t2[:], t2[:], K[:], ALU.mult)
            p.tt(t2[:], t2[:], R[:], ALU.mult)
            p.tt(t2[:], t2[:], VEC[:, 5, :], ALU.mult)
            p.reduce(small[:, 2, :], h3(t2), ALU.add)
            p.tt(h3(BN), h3(V), small.raw(small.t[:, 2, :].unsqueeze(2).to_broadcast([128, 8, 64])), ALU.mult)
            for sx, src in zip(SX, (R, Wd, Kd, Aa, Bb)):
                dst = sx.raw(sx.t.ap()[half * 8:(half + 1) * 8, s0:s0 + 128, :].rearrange("h t n -> t h n"))
                p.dma("sp", dst, h3(src))
            v3 = V.t[:].rearrange("p (q i) -> p q i", i=8)
            for qq in range(4):
                dstv = Sv2.raw(Sv2.t.ap()[half * 64 + qq * 16:half * 64 + (qq + 1) * 16, s0:s0 + 128, :].rearrange("q t i -> t q i"))
                p.dma("sp", dstv, V.raw(v3[:, qq * 16:(qq + 1) * 16, :]))
            p.dma("sp", gout[s0:s0 + 128, c0:c0 + 512], G[:])
            p.dma("sp", bout[s0:s0 + 128, c0:c0 + 512], BN[:])
            p.dma("sp", vout[s0:s0 + 128, c0:c0 + 512], V[:])

    SS = [p.sb([128, 8, 64], F32, f"S{i}") for i in range(2)]
    Sp = p.sb([128, 8, 64], F32, "Sp")
    T1 = p.sb([128, 8, 2, 64], F32, "T1")
    T2 = p.sb([128, 8, 64], F32, "T2")
    INB = [[p.sb([128, TC, 64], F32, f"in{i}{j}") for j in range(5)] for i in range(2)]
    VC = [p.sb([128, TC, 8], F32, f"vc{i}") for i in range(2)]
    RA = [p.sb([128, TC, 2, 64], F32, f"ra{i}") for i in range(2)]
    VK = [p.sb([128, TC, 8, 64], F32, f"vk{i}") for i in range(2)]
    OUTB = [p.sb([128, TC, 8, 2], F32, f"outb{i}") for i in range(2)]
    OC = [p.sb([128, TC, 8], F32, f"oc{i}") for i in range(2)]
    p.memset(SS[0][:], 0.0)
    step_no = [0]

    def bsrc(sx, t0, n):
        return sx.raw(bass.AP(sx.t, t0 * 64, [[T * 64, NH], [0, 8], [1, n * 64]]))

    nchunks = T // TC

    def load(ci):
        b = ci % 2
        t0 = ci * TC
        last = ci == nchunks
        Rc, Wc, Kc, Ac, Bc = INB[b]
        if ci == 0:
            p.memset(Rc[:, 0:1, :], 0.0)
            p.dma("sp", Rc[:, 1:TC, :].re("p t n -> p (t n)"), bsrc(SX[0], 0, TC - 1))
        elif not last:
            p.dma("sp", Rc[:].re("p t n -> p (t n)"), bsrc(SX[0], t0 - 1, TC))
        else:
            p.dma("sp", Rc[:, 0:1, :].re("p t n -> p (t n)"), bsrc(SX[0], t0 - 1, 1))
        if not last:
            for sx, dstt in zip(SX[1:], (Wc, Kc, Ac, Bc)):
                p.dma("sp", dstt[:].re("p t n -> p (t n)"), bsrc(sx, t0, TC))
            p.dma("sp", VC[b][:], Sv2[:, t0:t0 + TC, :])

    def prep(ci):
        b = ci % 2
        last = ci == nchunks
        Rc, Wc, Kc, Ac, Bc = INB[b]
        ra, vk, vc = RA[b], VK[b], VC[b]
        p.copy(ra[:, :, 0, :], Rc[:], e="act")
        if not last:
            p.copy(ra[:, :, 1, :], Ac[:], e="act")
            p.tt(vk[:], vc.raw(vc.t[:].unsqueeze(3).to_broadcast([128, TC, 8, 64])),
                 Kc.raw(Kc.t[:].unsqueeze(2).to_broadcast([128, TC, 8, 64])), ALU.mult, e="pool")

    load(0)
    prep(0)
    for ci in range(nchunks + 1):
        b = ci % 2
        t0 = ci * TC
        last = ci == nchunks
        Rc, Wc, Kc, Ac, Bc = INB[b]
        ra, vk, outb, oc, vc = RA[b], VK[b], OUTB[b], OC[b], VC[b]
        nsteps = TC if not last else 1
        if not last:
            load(ci + 1)
        for j in range(nsteps):
            S = SS[step_no[0] % 2]
            Sn = SS[(step_no[0] + 1) % 2]
            s_b = S.raw(S.t[:].unsqueeze(2).to_broadcast([128, 8, 2, 64]))
            ra_b = ra.raw(ra.t[:, j, :, :].unsqueeze(1).to_broadcast([128, 8, 2, 64]))
            p.tt(T1[:], s_b, ra_b, ALU.mult)
            if not last:
                p.tt(Sp[:], S[:], Wc.raw(Wc.t[:, j, :].unsqueeze(1).to_broadcast([128, 8, 64])), ALU.mult, e="pool")
            p.reduce(outb[:, j, :, :], T1[:], ALU.add)
            if not last:
                sa_b = outb.raw(outb.t[:, j, :, 1].unsqueeze(2).to_broadcast([128, 8, 64]))
                p.tt(T2[:], sa_b, Bc.raw(Bc.t[:, j, :].unsqueeze(1).to_broadcast([128, 8, 64])), ALU.mult)
                p.tt(T2[:], T2[:], vk[:, j, :, :], ALU.add)
                p.tt(Sn[:], Sp[:], T2[:], ALU.add)
                step_no[0] += 1
            if j == 1 and not last:
                prep(ci + 1)
        p.copy(oc[:, 0:nsteps, :], outb[:, 0:nsteps, :, 0], e="act")
        if ci == 0:
            p.dma("sp", O2[:, 0:TC - 1, :], oc[:, 1:TC, :])
        else:
            p.dma("sp", O2[:, t0 - 1:t0 - 1 + nsteps, :], oc[:, 0:nsteps, :])
    p.wait_all("sp", [O2[:], gout[:], bout[:], vout[:]])
    print("RWKV instr", p.ninst, "sems", p.nsem, "sbuf left", nc.sbuf_bytes_remaining)
    return nc


NCORES = 8
_cache = {}


def get_nc(key, fn, *a):
    if key not in _cache:
        _cache[key] = fn(*a)
    return _cache[key]


def run(nc, in_maps):
    res = run_bass_kernel_spmd(nc, in_maps, core_ids=list(range(NCORES)))
    return res.results


def build_mods():
    nc = bass.Bass("TRN2", target_bir_lowering=False)
    p = Prog(nc)
    condT = p.dram("condT", [128, KC, 3], F32, kind="ExternalInput")
    w = p.dram("w", [D, 6144], F32, kind="ExternalInput")
    b = p.dram("b", [128, 48], F32, kind="ExternalInput")
    o = p.dram("o", [128, 48, 3], F32, kind="ExternalOutput")
    cS = p.sb([128, KC, 3], F32, "cS")
    bS = p.sb([128, 48], F32, "bS")
    oS = p.sb([128, 48, 3], F32, "oS")
    W = [p.sb([128, KC, 512], F32, f"W{i}") for i in range(2)]
    PS = [p.ps([128, 512], F32, f"ps{i}") for i in range(2)]
    p.dma("sp", cS[:], condT[:])
    p.dma("sp", bS[:], b[:])
    p.act(cS[:], cS[:], AF.Silu)
    wv = w.raw(w.t.ap().rearrange("(kc p) c -> p kc c", p=128))
    for j in range(12):
        wb = W[j % 2]
        p.dma("sp", wb[:], wv[:, :, j * 512:(j + 1) * 512])
        for fl in range(4):
            ch = j * 4 + fl
            ps = PS[ch % 2]
            for kc in range(KC):
                p.mm(ps[:, 0:3], wb[:, kc, fl * 128:(fl + 1) * 128], cS[:, kc, :], start=(kc == 0), stop=(kc == KC - 1))
            p.ts(oS[:, ch, :], ps[:, 0:3], bS[:, ch:ch + 1], ALU.add)
    p.dma("sp", o[:], oS[:])
    p.wait_all("sp", [o[:]])
    return nc


def run_mods(c, c_ctx, ada_w, ada_b):
    cond = np.stack([c[0], c[1], c_ctx], 0)
    condT = np.ascontiguousarray(cond.T.reshape(KC, 128, 3).transpose(1, 0, 2))
    maps = []
    for core in range(NCORES):
        li, half = core // 2, core % 2
        wsl = np.ascontiguousarray(ada_w[li][:, half * 6144:(half + 1) * 6144])
        bsl = np.ascontiguousarray(ada_b[li][half * 6144:(half + 1) * 6144].reshape(48, 128).T)
        maps.append({"condT": condT, "w": wsl, "b": bsl})
    res = run(get_nc("mods", build_mods), maps)
    mods = []
    for li in range(4):
        parts = []
        for half in range(2):
            o = res[li * 2 + half]["o"]
            parts.append(o.transpose(2, 1, 0).reshape(3, 6144))
        mods.append(np.concatenate(parts, axis=1))
    return mods


def core_tokens(core):
    b, q = core // 4, core % 4
    return b, q * 2048, q * 64


def shard_fm(lat, ctx):
    out = []
    for core in range(NCORES):
        b, l0, c0 = core_tokens(core)
        out.append(np.ascontiguousarray(np.concatenate([lat[b, l0:l0 + 2048], ctx[b, c0:c0 + 64]], axis=0).T))
    return out


def unshard_fm(parts):
    C = parts[0].shape[0]
    lat = np.empty((2, 8192, C), np.float32)
    ctx = np.empty((2, 256, C), np.float32)
    for core in range(NCORES):
        b, l0, c0 = core_tokens(core)
        t = parts[core].T
        lat[b, l0:l0 + 2048] = t[:2048]
        ctx[b, c0:c0 + 64] = t[2048:]
    return lat, ctx


def col16(v):
    return np.ascontiguousarray(v.reshape(KC, 128).T)


def mod_core(modl, core):
    b = core // 4
    m = np.stack([modl[b], modl[2]], axis=-1)
    return np.ascontiguousarray(m.reshape(96, 128, 2).transpose(1, 0, 2))


IDENT = np.eye(128, dtype=np.float32)


def run_lb(kind, li, x_lat, x_ctx, mix_in, wo, bo, modl, inp, dbg=None):
    xs = shard_fm(x_lat, x_ctx)
    mi = {k: shard_fm(*v) for k, v in mix_in.items()}
    lngb = np.ascontiguousarray(np.stack([col16(inp["ln_g"][li, 0]), col16(inp["ln_b"][li, 0]),
                                          col16(inp["ln_g"][li, 1]), col16(inp["ln_b"][li, 1])], axis=1))
    b_in = np.ascontiguousarray(inp["moe_b_in"][li].reshape(NE, 12, 128).transpose(2, 0, 1))
    common = {"wo": wo, "bo": col16(bo), "lngb": lngb, "rw": inp["router_w"][li], "rb": inp["router_b"][li],
              "w_in": inp["moe_w_in"][li][:1 if dbg else NE], "b_in": b_in, "w_out": inp["moe_w_out"][li][:1 if dbg else NE],
              "b_out": inp["moe_b_out"][li], "ident": IDENT}
    if kind == "rwkv":
        j = li // 3
        common["lnx"] = np.ascontiguousarray(np.stack([col16(inp["rwkv_lnx_g"][j]), col16(inp["rwkv_lnx_b"][j])], axis=1))
    maps = []
    for core in range(NCORES):
        m = dict(common)
        m["xT"] = xs[core]
        m["mod"] = mod_core(modl, core)
        for k in mi:
            m[k] = mi[k][core]
        maps.append(m)
    res = run(get_nc(("lb", kind, dbg), build_lb, kind, dbg), maps)
    return unshard_fm([r["xo"] for r in res])


def full_fm(lat, ctx, b):
    return np.ascontiguousarray(np.concatenate([ctx[b], lat[b]], axis=0).T)


def run_lru(j, x_lat, x_ctx, modl, inp):
    maps = []
    xf = [full_fm(x_lat, x_ctx, b) for b in range(2)]
    for core in range(NCORES):
        b, cq = core // 4, core % 4
        sl = slice(cq * 512, (cq + 1) * 512)
        w = np.ascontiguousarray(np.concatenate([inp["lru_w_in"][j][:, sl], inp["lru_w_in"][j][:, 2048 + cq * 512:2048 + (cq + 1) * 512]], axis=1))
        convw = np.ascontiguousarray(inp["lru_conv_w"][j][:, sl].reshape(4, 4, 128).transpose(2, 1, 0))
        convb = np.ascontiguousarray(inp["lru_conv_b"][j][sl].reshape(4, 128).T)
        gw = np.ascontiguousarray(inp["lru_gate_w"][j][:, :, 2 * cq:2 * cq + 2])
        gb = np.ascontiguousarray(inp["lru_gate_b"][j][:, :, sl].reshape(2, 2, 4, 128).transpose(3, 0, 1, 2))
        lam = np.ascontiguousarray(inp["lru_lambda"][j][:, sl].reshape(2, 4, 128).transpose(2, 0, 1))
        maps.append({"xT": xf[b], "mod": mod_core(modl, core), "w": w, "convw": convw, "convb": convb,
                     "gw": gw, "gb": gb, "lam": lam})
    res = run(get_nc("lru", build_lru), maps)
    y_lat = np.empty((2, 8192, D), np.float32)
    y_ctx = np.empty((2, 256, D), np.float32)
    for core in range(NCORES):
        b, cq = core // 4, core % 4
        t = res[core]["yl"].T
        y_ctx[b, :, cq * 512:(cq + 1) * 512] = t[:256]
        y_lat[b, :, cq * 512:(cq + 1) * 512] = t[256:]
    return y_lat, y_ctx


def rope_tables():
    half = 32
    inv = (10000.0 ** (-np.arange(0, half, 2, dtype=np.float32) / half)).astype(np.float32)
    row = np.repeat(np.arange(128, dtype=np.float32), 64)
    col = np.tile(np.arange(64, dtype=np.float32), 128)
    ang_r = row[:, None] * inv[None, :]
    ang_c = col[:, None] * inv[None, :]
    ang = np.concatenate([ang_r, ang_r, ang_c, ang_c], axis=-1).astype(np.float32)
    return np.cos(ang).astype(np.float32), np.sin(ang).astype(np.float32)


ROT_PERM = np.concatenate([np.arange(16, 32), np.arange(0, 16), np.arange(48, 64), np.arange(32, 48)])
ROT_SIGN = np.concatenate([-np.ones(16), np.ones(16), -np.ones(16), np.ones(16)]).astype(np.float32)


def run_att(j, x_lat, x_ctx, modl, inp):
    cos, sin = rope_tables()
    cosT = np.ascontiguousarray(cos.T)
    sinT = np.ascontiguousarray((sin * ROT_SIGN[None, :]).T)
    masks = np.stack([np.tril(np.ones((128, 128), np.float32)), np.triu(np.ones((128, 128), np.float32))], axis=1)
    W = inp["attn_w_qkv"][j]
    B = inp["attn_b_qkv"][j]
    xf = [full_fm(x_lat, x_ctx, b) for b in range(2)]
    maps = []
    for core in range(NCORES):
        b, g = core // 4, core % 4
        qc = np.arange(g * 512, (g + 1) * 512)
        qcp = (qc // 64) * 64 + ROT_PERM[qc % 64]
        kc_ = 2048 + g * 64 + np.arange(64)
        kcp = 2048 + g * 64 + ROT_PERM
        vc = 2048 + 256 + g * 64 + np.arange(64)
        wq = np.ascontiguousarray(np.concatenate([W[:, qc], W[:, qcp]], axis=1))
        wk = np.ascontiguousarray(np.concatenate([W[:, kc_], W[:, kcp], W[:, vc]], axis=1))
        bq = np.ascontiguousarray(np.concatenate([B[qc].reshape(8, 64).T, B[qcp].reshape(8, 64).T], axis=1))
        bk = np.ascontiguousarray(np.stack([B[kc_], B[kcp]], axis=1))
        maps.append({"xT": xf[b], "mod": mod_core(modl, core), "wq": wq, "wk": wk, "bq": bq, "bk": bk,
                     "bv": np.ascontiguousarray(B[vc]), "cosT": cosT, "sinT": sinT,
                     "sink": np.ascontiguousarray(inp["attn_sink"][j][g * 8:(g + 1) * 8]), "masks": masks})
    res = run(get_nc("att", build_att), maps)
    y_lat = np.empty((2, 8192, D), np.float32)
    y_ctx = np.empty((2, 256, D), np.float32)
    for core in range(NCORES):
        b, g = core // 4, core % 4
        t = res[core]["o"]
        y_ctx[b, :, g * 512:(g + 1) * 512] = t[:256]
        y_lat[b, :, g * 512:(g + 1) * 512] = t[256:]
    return y_lat, y_ctx


def rwkv_core(core):
    return core // 4, (core // 2) % 2, core % 2


def seq_order(lat, ctx, b, z):
    if z == 0:
        return np.concatenate([ctx[b], lat[b]], axis=0)
    return np.concatenate([ctx[b][::-1], lat[b][::-1]], axis=0)


def unseq(a, z):
    c, l = a[:256], a[256:]
    if z == 1:
        c, l = c[::-1], l[::-1]
    return c, l


def run_rwkv(j, x_lat, x_ctx, modl, inp, vfirst):
    has_vres = j > 0
    maps = []
    for core in range(NCORES):
        b, z, hq = rwkv_core(core)
        chs = slice(hq * 1024, (hq + 1) * 1024)
        W = inp["rwkv_w_rkv"][j]
        wrkv = np.stack([np.concatenate([W[m][:, hq * 1024 + h * 512:hq * 1024 + (h + 1) * 512] for m in range(3)], axis=1)
                         for h in range(2)], axis=0)
        parts = [inp["rwkv_w1"][j][z], inp["rwkv_a1"][j][z], inp["rwkv_a1"][j][1 - z], inp["rwkv_g1"][j]]
        if has_vres:
            parts.append(inp["rwkv_v1"][j - 1])
        vec = [inp["rwkv_w0"][j][z][chs], inp["rwkv_a0"][j][z][chs], inp["rwkv_a0"][j][1 - z][chs],
               inp["rwkv_k_k"][j][chs], inp["rwkv_k_a"][j][chs], inp["rwkv_r_k"][j].reshape(-1)[chs],
               inp["rwkv_v0"][j - 1][chs] if has_vres else np.zeros(1024, np.float32)]
        m = {"xT": np.ascontiguousarray(seq_order(x_lat, x_ctx, b, z).T), "mod": mod_core(modl, core),
             "mix": np.ascontiguousarray(inp["rwkv_mix"][j].reshape(6, KC, 128).transpose(2, 1, 0)),
             "wrkv": np.ascontiguousarray(wrkv), "wl1": np.ascontiguousarray(np.concatenate(parts, axis=1)),
             "w2": np.ascontiguousarray(inp["rwkv_w2"][j][z][:, chs]),
             "a2": np.ascontiguousarray(np.stack([inp["rwkv_a2"][j][z][:, chs], inp["rwkv_a2"][j][1 - z][:, chs]])),
             "g2": np.ascontiguousarray(inp["rwkv_g2"][j][:, chs]), "vecs": np.ascontiguousarray(np.stack(vec))}
        if has_vres:
            m["v2"] = np.ascontiguousarray(inp["rwkv_v2"][j - 1][:, chs])
            m["vf"] = vfirst[core]
        maps.append(m)
    res = run(get_nc(("rwkv", has_vres), build_rwkv, has_vres), maps)
    names = ("of", "ob", "gg", "bn")
    lat = {n: np.zeros((2, 8192, D), np.float32) for n in names}
    ctx = {n: np.zeros((2, 256, D), np.float32) for n in names}
    for core in range(NCORES):
        b, z, hq = rwkv_core(core)
        chs = slice(hq * 1024, (hq + 1) * 1024)
        o = res[core]["O2"].reshape(16, 8, T_ALL, 8).transpose(2, 0, 1, 3).reshape(T_ALL, 1024)
        c_, l_ = unseq(o, z)
        key = "of" if z == 0 else "ob"
        ctx[key][b, :, chs], lat[key][b, :, chs] = c_, l_
        if z == 0:
            for key, nm in (("gg", "gout"), ("bn", "bout")):
                c_, l_ = unseq(res[core][nm], 0)
                ctx[key][b, :, chs], lat[key][b, :, chs] = c_, l_
    vf_new = [res[core]["vout"] for core in range(NCORES)]
    return {n + "T": (lat[n], ctx[n]) for n in names}, vf_new


def kernel(**inputs):
    inp = {k: np.asarray(v) for k, v in inputs.items()}
    mods = run_mods(inp["c"], inp["c_ctx"], inp["ada_w"], inp["ada_b"])
    x_lat = np.ascontiguousarray(inp["x"], dtype=np.float32)
    x_ctx = np.ascontiguousarray(inp["ctx"], dtype=np.float32)
    zeros = np.zeros(D, np.float32)
    vf = None
    for i in range(4):
        kind, j = i % 3, i // 3
        if kind == 0:
            mi, vf_new = run_rwkv(j, x_lat, x_ctx, mods[i], inp, vf)
            if j == 0:
                vf = vf_new
            x_lat, x_ctx = run_lb("rwkv", i, x_lat, x_ctx, mi, inp["rwkv_w_o"][j], zeros, mods[i], inp)
        elif kind == 1:
            y_lat, y_ctx = run_lru(j, x_lat, x_ctx, mods[i], inp)
            x_lat, x_ctx = run_lb("plain", i, x_lat, x_ctx, {"yT": (y_lat, y_ctx)}, inp["lru_w_out"][j], zeros, mods[i], inp)
        else:
            y_lat, y_ctx = run_att(j, x_lat, x_ctx, mods[i], inp)
            x_lat, x_ctx = run_lb("plain", i, x_lat, x_ctx, {"yT": (y_lat, y_ctx)}, inp["attn_w_o"][j], inp["attn_b_o"][j], mods[i], inp)
    return x_lat.astype(np.float32)
```
